# Optimizing a Trainium2 kernel written in Bass

```python
import jax, jax.numpy as jnp
from jax import lax
import numpy as np

D_MODEL = 1024
BATCH = 8
SEQ = 2048
DEPTH = 1

A_HEADS = 8
A_HEAD_DIM = 64
IDX_HEADS = 8
IDX_DIM = 64
TOPK_MAX = 256
B_HEADS = 8
Q_LORA = 384
KV_LORA = 256
QK_NOPE = 64
QK_ROPE = 32
V_DIM = 64
ROPE_THETA = 10000.0
Q_BLOCK = 128
N_EXPERTS = 32
TOP_K = 4
D_FF = 1024
SWIGLU_LIMIT = 7.0
SWIGLU_ALPHA = 1.702
DN_ALPHA = (2 * DEPTH) ** 0.25
DN_BETA = (8 * DEPTH) ** -0.25
LN_EPS = 1e-5
RMS_EPS = 1e-6

A_WIDTH = A_HEADS * A_HEAD_DIM
B_WIDTH = B_HEADS * V_DIM
SPLITS = (A_WIDTH, A_WIDTH, A_WIDTH, IDX_HEADS * IDX_DIM, IDX_DIM, IDX_HEADS,
          Q_LORA, KV_LORA, QK_ROPE, 2 * D_MODEL)
D_IN = sum(SPLITS)

kernel_name = "hybrid_dsa_mla_gated_moe_deepnorm"


def layer_norm(x, g, b):
    xf = x.astype(jnp.float32)
    mu = jnp.mean(xf, axis=-1, keepdims=True)
    var = jnp.mean(jnp.square(xf - mu), axis=-1, keepdims=True)
    return ((xf - mu) * lax.rsqrt(var + LN_EPS) * g + b).astype(x.dtype)


def rms_norm(x, g):
    xf = x.astype(jnp.float32)
    return (xf * lax.rsqrt(jnp.mean(jnp.square(xf), -1, keepdims=True) + RMS_EPS) * g).astype(x.dtype)


def rope(x, positions):
    half = x.shape[-1] // 2
    inv_freq = ROPE_THETA ** (-jnp.arange(half, dtype=jnp.float32) / half)
    ang = positions.astype(jnp.float32)[..., None] * inv_freq
    ang = ang.reshape(ang.shape[:2] + (1,) * (x.ndim - 3) + (half,))
    cos, sin = jnp.cos(ang).astype(x.dtype), jnp.sin(ang).astype(x.dtype)
    x1, x2 = x[..., :half], x[..., half:]
    return jnp.concatenate([x1 * cos - x2 * sin, x1 * sin + x2 * cos], axis=-1)


def sweep_query_blocks(block_fn, seq):
    out = lax.map(block_fn, jnp.arange(seq // Q_BLOCK) * Q_BLOCK)
    out = jnp.moveaxis(out, 0, 1)
    return out.reshape((out.shape[0], seq) + out.shape[3:])


def dsa_attention(q, k, v, q_idx, k_idx, w_idx):
    seq = q.shape[1]
    k_sel = min(TOPK_MAX, seq // 4)
    key_pos = jnp.arange(seq)
    scale = A_HEAD_DIM ** -0.5
    idx_scale = IDX_DIM ** -0.5
    w_scale = IDX_HEADS ** -0.5

    def block(start):
        qb = lax.dynamic_slice_in_dim(q, start, Q_BLOCK, axis=1)
        qib = lax.dynamic_slice_in_dim(q_idx, start, Q_BLOCK, axis=1)
        wb = lax.dynamic_slice_in_dim(w_idx, start, Q_BLOCK, axis=1)
        q_pos = start + jnp.arange(Q_BLOCK)
        causal = key_pos[None, :] <= q_pos[:, None]
        rel = jax.nn.relu(jnp.einsum('bthd,bsd->bths', qib, k_idx).astype(jnp.float32) * idx_scale)
        score = jnp.einsum('bths,bth->bts', rel, wb.astype(jnp.float32) * w_scale)
        score = jnp.where(causal[None], score, -jnp.inf)
        _, sel = lax.top_k(score, k_sel)
        valid = sel <= q_pos[None, :, None]
        k_g = jax.vmap(lambda kb, ib: kb[ib])(k, sel)
        v_g = jax.vmap(lambda vb, ib: vb[ib])(v, sel)
        logits = jnp.einsum('bthd,btkhd->bhtk', qb, k_g).astype(jnp.float32) * scale
        logits = jnp.where(valid[:, None], logits, -jnp.inf)
        p = jax.nn.softmax(logits, axis=-1).astype(v.dtype)
        return jnp.einsum('bhtk,btkhd->bthd', p, v_g)

    return sweep_query_blocks(block, seq)


def mla_attention(q_nope, q_rope, k_nope, k_rope, v):
    seq = q_nope.shape[1]
    key_pos = jnp.arange(seq)
    scale = (QK_NOPE + QK_ROPE) ** -0.5

    def block(start):
        qn = lax.dynamic_slice_in_dim(q_nope, start, Q_BLOCK, axis=1)
        qr = lax.dynamic_slice_in_dim(q_rope, start, Q_BLOCK, axis=1)
        q_pos = start + jnp.arange(Q_BLOCK)
        causal = key_pos[None, :] <= q_pos[:, None]
        logits = (jnp.einsum('bthd,bshd->bhts', qn, k_nope)
                  + jnp.einsum('bthd,bsd->bhts', qr, k_rope)).astype(jnp.float32) * scale
        logits = jnp.where(causal[None, None], logits, -jnp.inf)
        p = jax.nn.softmax(logits, axis=-1).astype(v.dtype)
        return jnp.einsum('bhts,bshd->bthd', p, v)

    return sweep_query_blocks(block, seq)


def routed_experts(h, w_router, b_router, w_up, b_up, w_down, b_down):
    bsz, seq, d = h.shape
    t = h.reshape(-1, d)
    logits = (t @ w_router + b_router).astype(jnp.float32)
    top_vals, top_idx = lax.top_k(logits, TOP_K)
    gates = jax.nn.softmax(top_vals, axis=-1)
    combine = jnp.sum(jax.nn.one_hot(top_idx, N_EXPERTS, dtype=jnp.float32) * gates[..., None], axis=1)

    def expert(acc, p):
        wu, bu, wd, bd, c = p
        a = t @ wu + bu
        g = jnp.minimum(a[:, :D_FF], SWIGLU_LIMIT)
        lin = jnp.clip(a[:, D_FF:], -SWIGLU_LIMIT, SWIGLU_LIMIT)
        y = ((lin + 1.0) * (g * jax.nn.sigmoid(SWIGLU_ALPHA * g))) @ wd + bd
        return acc + c[:, None].astype(t.dtype) * y, None

    acc, _ = lax.scan(expert, jnp.zeros_like(t), (w_up, b_up, w_down, b_down, combine.T))
    return acc.reshape(bsz, seq, d)


def setup_inputs(seed: int = 0) -> dict:
    key = jax.random.key(seed)
    ks = jax.random.split(key, 24)
    f32 = jnp.float32
    L = DEPTH

    def nrm(k, shape, scale):
        return jax.random.normal(k, shape, f32) * scale

    x = nrm(ks[0], (BATCH, SEQ, D_MODEL), 1.0)
    start = jax.random.randint(ks[1], (BATCH, 1), 0, 4096, dtype=jnp.int32)
    positions = start + jnp.arange(SEQ, dtype=jnp.int32)[None, :]
    return {
        "x": x,
        "positions": positions,
        "w_in": nrm(ks[2], (L, D_MODEL, D_IN), D_MODEL ** -0.5),
        "b_gate": nrm(ks[3], (L, 2 * D_MODEL), 0.1),
        "rms_cq": 1.0 + nrm(ks[4], (L, Q_LORA), 0.01),
        "rms_ckv": 1.0 + nrm(ks[5], (L, KV_LORA), 0.01),
        "w_uq": nrm(ks[6], (L, Q_LORA, B_HEADS * (QK_NOPE + QK_ROPE)), Q_LORA ** -0.5),
        "w_ukv": nrm(ks[7], (L, KV_LORA, B_HEADS * (QK_NOPE + V_DIM)), KV_LORA ** -0.5),
        "w_o_a": nrm(ks[8], (L, A_WIDTH, D_MODEL), A_WIDTH ** -0.5),
        "w_o_b": nrm(ks[9], (L, B_WIDTH, D_MODEL), B_WIDTH ** -0.5),
        "w_out": nrm(ks[10], (L, D_MODEL, D_MODEL), D_MODEL ** -0.5 * DN_BETA),
        "ln1_g": 1.0 + nrm(ks[11], (L, D_MODEL), 0.01),
        "ln1_b": nrm(ks[12], (L, D_MODEL), 0.01),
        "w_router": nrm(ks[13], (L, D_MODEL, N_EXPERTS), D_MODEL ** -0.5),
        "b_router": nrm(ks[14], (L, N_EXPERTS), 0.01),
        "w_up": nrm(ks[15], (L, N_EXPERTS, D_MODEL, 2 * D_FF), D_MODEL ** -0.5),
        "b_up": nrm(ks[16], (L, N_EXPERTS, 2 * D_FF), 0.01),
        "w_down": nrm(ks[17], (L, N_EXPERTS, D_FF, D_MODEL), D_FF ** -0.5 * DN_BETA),
        "b_down": nrm(ks[18], (L, N_EXPERTS, D_MODEL), 0.01),
        "ln2_g": 1.0 + nrm(ks[19], (L, D_MODEL), 0.01),
        "ln2_b": nrm(ks[20], (L, D_MODEL), 0.01),
    }


def reference(x, positions, w_in, b_gate, rms_cq, rms_ckv, w_uq, w_ukv, w_o_a, w_o_b,
              w_out, ln1_g, ln1_b, w_router, b_router, w_up, b_up, w_down, b_down,
              ln2_g, ln2_b):
    bsz, seq, _ = x.shape
    offsets = [int(o) for o in np.cumsum(SPLITS)[:-1]]
    for l in range(DEPTH):
        z = x @ w_in[l]
        qa, ka, va, qi, ki, wi, cq, ckv, kr, gates = jnp.split(z, offsets, axis=-1)
        qa = rope(qa.reshape(bsz, seq, A_HEADS, A_HEAD_DIM), positions)
        ka = rope(ka.reshape(bsz, seq, A_HEADS, A_HEAD_DIM), positions)
        va = va.reshape(bsz, seq, A_HEADS, A_HEAD_DIM)
        qi = rope(qi.reshape(bsz, seq, IDX_HEADS, IDX_DIM), positions)
        ki = rope(ki, positions)
        o_a = dsa_attention(qa, ka, va, qi, ki, wi).reshape(bsz, seq, A_WIDTH)
        qb = (rms_norm(cq, rms_cq[l]) @ w_uq[l]).reshape(bsz, seq, B_HEADS, QK_NOPE + QK_ROPE)
        q_nope, q_rope = qb[..., :QK_NOPE], rope(qb[..., QK_NOPE:], positions)
        kv = (rms_norm(ckv, rms_ckv[l]) @ w_ukv[l]).reshape(bsz, seq, B_HEADS, QK_NOPE + V_DIM)
        k_nope, v_b = kv[..., :QK_NOPE], kv[..., QK_NOPE:]
        k_rope = rope(kr, positions)
        o_b = mla_attention(q_nope, q_rope, k_nope, k_rope, v_b).reshape(bsz, seq, B_WIDTH)
        g = jax.nn.sigmoid(gates + b_gate[l]).reshape(bsz, seq, 2, D_MODEL)
        mix = g[:, :, 0] * (o_a @ w_o_a[l]) + g[:, :, 1] * (o_b @ w_o_b[l])
        h = layer_norm(DN_ALPHA * x + mix @ w_out[l], ln1_g[l], ln1_b[l])
        ffn = routed_experts(h, w_router[l], b_router[l], w_up[l], b_up[l], w_down[l], b_down[l])
        x = layer_norm(DN_ALPHA * h + ffn, ln2_g[l], ln2_b[l])
    return x
```

```python
import numpy as np
from contextlib import ExitStack
import concourse.bass as bass
import concourse.mybir as mybir
from concourse.bass_utils import run_bass_kernel_spmd

F32 = mybir.dt.float32
BF16 = mybir.dt.bfloat16
I32 = mybir.dt.int32
ALU = mybir.AluOpType
AF = mybir.ActivationFunctionType
AXX = mybir.AxisListType.X

S = 2048
D = 1024
NCORES = 8
E = 32
DFF = 1024
OFF_QA, OFF_KA, OFF_VA, OFF_QI, OFF_KI, OFF_WI, OFF_CQ, OFF_CKV, OFF_KR, OFF_G = (
    0, 512, 1024, 1536, 2048, 2112, 2120, 2504, 2760, 2792)
D_IN = 4840
NEG = -30000.0
TWO_PI = float(2 * np.pi)
DN_ALPHA = float(2.0 ** 0.25)
N_BISECT = 20
import os
KSUB = int(os.environ.get('KSUB', '9'))
KLAT = int(os.environ.get('KLAT', '9'))
KSKIP1 = int(os.environ.get('KSKIP1', '0'))
COMPUTE = ("pe", "act", "dve", "pool")


class Rec:
    def __init__(self):
        self.call = None

    def __getattr__(self, name):
        def f(*a, **k):
            self.call = (name, a, k)
            return self
        return f


class Op:
    __slots__ = ("eng", "fn", "deps", "sig", "sigval", "dma_key", "dma_cnt")

    def __init__(self, eng, fn):
        self.eng = eng
        rec = Rec()
        fn(rec)
        self.fn = rec.call
        self.deps = {}
        self.sig = False
        self.sigval = 0
        self.dma_key = None
        self.dma_cnt = 0


class Sched:
    def __init__(self, nc, eng_sems, dma_sem_pool):
        self.nc = nc
        self.eng_sems = eng_sems
        self.pool = list(dma_sem_pool)
        self.cnt = {e: 0 for e in COMPUTE}
        self.dma_sem = {}
        self.dma_cnt = {}
        self.seen = {e: {} for e in ("pe", "act", "dve", "pool", "sp")}
        self.reset()

    def reset(self):
        self.ops = {e: [] for e in ("pe", "act", "dve", "pool", "sp")}
        self.lastw = {}
        self.reads = {}

    def _add(self, eng, fn, reads, writes, dma_key=None):
        op = Op(eng, fn)
        idx = len(self.ops[eng])
        if dma_key is not None:
            if dma_key not in self.dma_sem:
                self.dma_sem[dma_key] = self.pool.pop()
                self.dma_cnt[dma_key] = 0
            self.dma_cnt[dma_key] += 16
            op.dma_key = dma_key
            op.dma_cnt = self.dma_cnt[dma_key]
            mysrc, mytok = ("dma", dma_key), op.dma_cnt
        else:
            mysrc, mytok = eng, idx
        deps = {}

        def merge(d):
            for s, t in d.items():
                if s not in deps or deps[s] < t:
                    deps[s] = t

        for r in reads:
            merge(self.lastw.get(r, {}))
            if (r if isinstance(r, str) else r[0]).startswith("ps"):
                merge({s_: t_ for s_, t_ in self.reads.get(r, {}).items() if s_ != mysrc})
        for w in writes:
            merge(self.lastw.get(w, {}))
            merge(self.reads.get(w, {}))
        if dma_key is None and eng in deps:
            if eng == "pe" or deps[eng] < idx - 1:
                del deps[eng]
        if mysrc in deps and dma_key is not None:
            del deps[mysrc]
        op.deps = deps
        for r in reads:
            self.reads.setdefault(r, {})[mysrc] = mytok
        for w in writes:
            self.lastw[w] = {mysrc: mytok}
            self.reads[w] = {}
        self.ops[eng].append(op)
        return op

    def pe(self, fn, reads=(), writes=()):
        return self._add("pe", fn, reads, writes)

    def act(self, fn, reads=(), writes=()):
        return self._add("act", fn, reads, writes)

    def dve(self, fn, reads=(), writes=()):
        return self._add("dve", fn, reads, writes)

    def gp(self, fn, reads=(), writes=()):
        return self._add("pool", fn, reads, writes)

    def dma(self, eng, out, in_, key, reads=(), writes=()):
        return self._add(eng, lambda e: e.dma_start(out=out, in_=in_), reads, writes, dma_key=key)

    def flush(self):
        ops = self.ops
        for e in ops:
            for op in ops[e]:
                for s, t in op.deps.items():
                    if isinstance(s, str):
                        ops[s][t].sig = True
        final = {}
        for e in COMPUTE:
            last = None
            for op in ops[e]:
                if op.dma_key is None:
                    last = op
            if last is not None:
                last.sig = True
            run = self.cnt[e]
            for op in ops[e]:
                if op.dma_key is None and op.sig:
                    run += 1
                    op.sigval = run
            self.cnt[e] = run
            final[e] = run
        dma_final = dict(self.dma_cnt)

        def emit(ename, eobj):
            seen = self.seen[ename]
            for op in ops[ename]:
                for s, t in op.deps.items():
                    if isinstance(s, str):
                        val = ops[s][t].sigval
                        sem = self.eng_sems[s]
                    else:
                        val = dma_final[s[1]] if s[1] in ("c0", "c1") else t
                        sem = self.dma_sem[s[1]]
                    if seen.get(s, 0) < val:
                        eobj.wait_ge(sem, val)
                        seen[s] = val
                name_, a_, k_ = op.fn
                inst = getattr(eobj, name_)(*a_, **k_)
                if op.dma_key is not None:
                    inst.then_inc(self.dma_sem[op.dma_key], 16)
                elif op.sig:
                    inst.then_inc(self.eng_sems[ename], 1)
            for s in COMPUTE:
                if s != ename and seen.get(s, 0) < final[s]:
                    eobj.wait_ge(self.eng_sems[s], final[s])
                    seen[s] = final[s]
            for k, v in dma_final.items():
                s = ("dma", k)
                if seen.get(s, 0) < v:
                    eobj.wait_ge(self.dma_sem[k], v)
                    seen[s] = v

        with self.nc.Block() as blk:
            @blk.tensor
            def _(t):
                emit("pe", t)

            @blk.scalar
            def _(a):
                emit("act", a)

            @blk.vector
            def _(v):
                emit("dve", v)

            @blk.gpsimd
            def _(g):
                emit("pool", g)

            @blk.sync
            def _(sp):
                emit("sp", sp)
        self.reset()


class Stream:
    def __init__(self, sch, name, slots, loads, eng="pool"):
        self.sch, self.name, self.slots, self.loads, self.eng = sch, name, slots, loads, eng
        self.issued = 0
        self.consumed = 0

    def _issue(self, i):
        sl = i % len(self.slots)
        for (o, a) in self.loads[i](self.slots[sl]):
            self.sch.dma(self.eng, o, a, key=(self.name, sl), writes=[(self.name, sl)])

    def next(self):
        n = len(self.slots)
        while self.issued < min(len(self.loads), self.consumed + n):
            self._issue(self.issued)
            self.issued += 1
        sl = self.consumed % n
        self.consumed += 1
        return self.slots[sl], (self.name, sl)


class _Stop(Exception):
    pass


def build_program(dbg=None, stop_after=4):
    dbg = dbg or {}
    nc = bass.Bass("TRN2", target_bir_lowering=False)
    dt_ = {}

    def din(name, shape, dtype=F32):
        h = nc.dram_tensor(name, list(shape), dtype, kind="ExternalInput")
        dt_[name] = h
        return h

    x_h = din("x", [S, D])
    pos_h = din("pos", [1, S], I32)
    w_in_h = din("w_in", [D, D_IN])
    bgate_h = din("b_gate", [128, 16])
    rmscq_h = din("rms_cq", [128, 3])
    rmsckv_h = din("rms_ckv", [128, 2])
    wuq_h = din("w_uq", [384, 768])
    wukv_k_h = din("w_ukv_k", [256, 512])
    wukv_v_h = din("w_ukv_v", [256, 512])
    woa_h = din("w_o_a", [512, D])
    wob_h = din("w_o_b", [512, D])
    wout_h = din("w_out", [D, D])
    ln1g_h = din("ln1_g", [1, D])
    ln1b_h = din("ln1_b", [1, D])
    wr_h = din("w_router", [D, E])
    br_h = din("b_router", [1, E])
    wup_h = din("w_up", [E, D, 2 * DFF])
    bup_h = din("b_up", [128, E * 16])
    wdn_h = din("w_down", [E, DFF, D])
    bdn_h = din("b_down", [E, D])
    ln2g_h = din("ln2_g", [1, D])
    ln2b_h = din("ln2_b", [1, D])
    c_identb = din("c_identb", [128, 128])
    c_perm64 = din("c_perm64", [128, 128])
    c_perm16 = din("c_perm16", [128, 128])
    c_tri = din("c_tri", [128, 128])
    c_triT = din("c_triT", [128, 4 * 512])
    c_invf = din("c_invf", [128, 2])
    c_krow = din("c_krow", [128, 16])

    y_h = nc.dram_tensor("y", [S, D], F32, kind="ExternalOutput")
    hs_h = y_h
    dbg_out = {}
    for k, shp in dbg.items():
        dbg_out[k] = nc.dram_tensor("dbg_" + k, list(shp), F32, kind="ExternalOutput")

    es = ExitStack()
    try:
      with es:
          def sb(name, shape, dtype, stack=es):
              return stack.enter_context(nc.sbuf_tensor(name, list(shape), dtype))

          def psum(name, shape, dtype, stack):
              return stack.enter_context(nc.psum_tensor(name, list(shape), dtype))

          eng_sems = {e: es.enter_context(nc.semaphore("sem_" + e)) for e in COMPUTE}
          dma_pool = [es.enter_context(nc.semaphore("dsem%d" % i)) for i in range(40)]
          sch = Sched(nc, eng_sems, dma_pool)

          identb = sb("identb", [128, 128], BF16)
          identf = sb("identf", [128, 128], F32)
          perm64 = sb("perm64", [128, 128], F32)
          perm16 = sb("perm16", [128, 128], F32)
          onesb = sb("onesb", [128, 128], BF16)
          onesf = sb("onesf", [128, 128], F32)
          tri = sb("tri", [128, 128], F32)
          triT = sb("triT", [128, 4, 512], BF16)
          invf = sb("invf", [128, 2], F32)
          krow = sb("krow", [128, 16], F32)
          epsln = sb("epsln", [128, 1], F32)
          epsrms = sb("epsrms", [128, 1], F32)
          negpi = sb("negpi", [128, 1], F32)
          XH = sb("XH", [128, 8, S], BF16)
          comb = sb("comb", [128, 16, E], F32)

          sch.dma("pool", identb[:], c_identb.ap(), key="c0", writes=["identb"])
          sch.dma("sp", identf[:], c_identb.ap(), key="c1", writes=["identf"])
          sch.dma("sp", perm64[:], c_perm64.ap(), key="c1", writes=["perm64"])
          sch.dma("sp", perm16[:], c_perm16.ap(), key="c1", writes=["perm16"])
          sch.dma("sp", tri[:], c_tri.ap(), key="c1", writes=["tri"])
          sch.dma("pool", triT[:].rearrange("p a b -> p (a b)"), c_triT.ap(), key="c0", writes=["triT"])
          sch.dma("sp", invf[:], c_invf.ap(), key="c1", writes=["invf"])
          sch.dma("sp", krow[:], c_krow.ap(), key="c1", writes=["krow"])
          sch.dve(lambda v: v.memset(onesb[:], 1.0), writes=["onesb"])
          sch.dve(lambda v: v.memset(onesf[:], 1.0), writes=["onesf"])
          sch.dve(lambda v: v.memset(epsln[:], 1e-5), writes=["epsln"])
          sch.dve(lambda v: v.memset(epsrms[:], 1e-6), writes=["epsrms"])
          sch.dve(lambda v: v.memset(negpi[:], -float(np.pi)), writes=["negpi"])

          w_in_v = w_in_h.ap().rearrange("(kc p) n -> p kc n", p=128)

          def dump(name, src_ap, reads, rows=None):
              if name in dbg_out:
                  d = dbg_out[name].ap()
                  sch.dma("sp", d if rows is None else d[rows[0]:rows[1]], src_ap, key="dbg", reads=reads)

          def build_tables(tc, col, posi, posf, ang, u, ki, Ct, St, ukey="u", kikey="ki"):
              pos_bc = bass.AP(pos_h, tc * 512, [[0, 128], [1, 512]])
              sch.dma("sp", posi[:], pos_bc, key="pos", writes=["posi"])
              sch.dve(lambda v: v.tensor_copy(out=posf[:], in_=posi[:]), reads=["posi"], writes=["posf"])
              sch.dve(lambda v: v.tensor_scalar(out=ang[:], in0=posf[:], scalar1=invf[:, col:col + 1], scalar2=None,
                                                op0=ALU.mult), reads=["posf", "invf"], writes=["ang"])
              for (shift, T, nm) in ((0.0, St, "S"), (float(np.pi / 2), Ct, "C")):
                  sch.dve(lambda v, shift=shift: v.tensor_scalar(out=posf[:], in0=ang[:], scalar1=shift, scalar2=None,
                                                                 op0=ALU.add), reads=["ang", "posf"], writes=["posf"])
                  sch.dve(lambda v: v.tensor_scalar(out=u[:], in0=posf[:], scalar1=1.0 / TWO_PI, scalar2=None,
                                                    op0=ALU.mult), reads=["posf"], writes=[ukey])
                  sch.dve(lambda v: v.tensor_copy(out=ki[:], in_=u[:]), reads=[ukey], writes=[kikey])
                  sch.dve(lambda v: v.tensor_copy(out=u[:], in_=ki[:]), reads=[kikey], writes=[ukey])
                  sch.dve(lambda v: v.scalar_tensor_tensor(out=u[:], in0=u[:], scalar=-TWO_PI, in1=posf[:],
                                                           op0=ALU.mult, op1=ALU.add), reads=[ukey, "posf"], writes=[ukey])
                  sch.dve(lambda v: v.tensor_scalar(out=u[:], in0=u[:], scalar1=float(np.pi) - 1e-6,
                                                    scalar2=-float(np.pi) + 1e-6, op0=ALU.min, op1=ALU.max),
                          reads=[ukey], writes=[ukey])
                  sch.act(lambda a, T=T: a.activation(out=T[:], in_=u[:], func=AF.Sin), reads=[ukey], writes=[("tab", nm)])

          def rope_block(ps, ps_key, r0, r1, perm, permkey, psR, psR_key, zs, t1, Ct, St, dsts, dst_keys):
              sch.act(lambda a: a.copy(out=zs[r0:r1, :], in_=ps[r0:r1, :]), reads=[ps_key], writes=["zs"])
              sch.pe(lambda t: t.matmul(psR[:, :], lhsT=perm[:, :], rhs=zs[:, :], start=True, stop=True),
                     reads=["zs", permkey], writes=[psR_key])
              sch.dve(lambda v: v.tensor_tensor(out=t1[r0:r1, :], in0=zs[r0:r1, :], in1=Ct[r0:r1, :], op=ALU.mult),
                      reads=["zs", ("tab", "C")], writes=["t1"])
              sch.dve(lambda v: v.tensor_tensor(out=zs[r0:r1, :], in0=psR[r0:r1, :], in1=St[r0:r1, :], op=ALU.mult),
                      reads=[psR_key, ("tab", "S")], writes=["zs"])
              for d, dk in zip(dsts, dst_keys):
                  sch.dve(lambda v, d=d: v.tensor_tensor(out=d, in0=t1[r0:r1, :], in1=zs[r0:r1, :], op=ALU.add),
                          reads=["t1", "zs"], writes=[dk])

          def attn_chunk(nst, heads, psL, psO2, psD2, pT, rden, scale):
              steps = [(hi, st) for hi in range(len(heads)) for st in range(nst)]

              def emit_L(i):
                  hi, st = steps[i]
                  H = heads[hi]
                  L = psL[i % 2]
                  Lk = ("psG", i % 2)
                  b = H["bias_fn"](st)
                  sch.pe(lambda t: t.matmul(L[:, :], lhsT=H["kT_fn"](st), rhs=H["qT_fn"](), start=True, stop=(b is None)),
                         reads=H["kq_reads"], writes=[Lk])
                  if b is not None:
                      sch.pe(lambda t: t.matmul(L[:, :], lhsT=identb[:, :], rhs=b[0], start=False, stop=True),
                             reads=["identb"] + b[1], writes=[Lk])
                  P = pT[i % 3]
                  sch.act(lambda a: a.activation(out=P[:, :], in_=L[:, :], func=AF.Exp, scale=scale),
                          reads=[Lk], writes=[("pT", i % 3)])

              def emit_PV(i):
                  hi, st = steps[i]
                  H = heads[hi]
                  P = pT[i % 3]
                  Pk = ("pT", i % 3)
                  O = psO2[hi % 2]
                  Dn = psD2[hi % 2]
                  sch.pe(lambda t: t.matmul(O[:, :], lhsT=H["v_fn"](st), rhs=P[:, :], start=(st == 0), stop=(st == nst - 1)),
                         reads=[Pk] + H["v_reads"], writes=[("psO", hi % 2)])
                  sch.pe(lambda t: t.matmul(Dn[:, :], lhsT=onesb[:, :], rhs=P[:, :], start=(st == 0), stop=(st == nst - 1)),
                         reads=[Pk, "onesb"], writes=[("psD", hi % 2)])
                  if st == nst - 1:
                      r0, r1 = H["rows"]
                      sch.dve(lambda v: v.reciprocal(out=rden[r0:r1, :], in_=Dn[r0:r1, :]), reads=[("psD", hi % 2)],
                              writes=["rden"])
                      sch.dve(lambda v: v.tensor_tensor(out=H["out_ap"], in0=O[r0:r1, :], in1=rden[r0:r1, :], op=ALU.mult),
                              reads=[("psO", hi % 2), "rden"], writes=[H["out_key"]])

              for i in range(len(steps) + 1):
                  if i < len(steps):
                      emit_L(i)
                  if i >= 1:
                      emit_PV(i - 1)

          es_o = ExitStack()
          with es_o:
              oaT = sb("oaT", [128, 4, S], BF16, es_o)
              obT = sb("obT", [128, 4, S], BF16, es_o)

              p1 = ExitStack()
              with p1:
                  KA = sb("KA", [128, 4, S], BF16, p1)
                  KI2 = sb("KI2", [128, S], BF16, p1)
                  VA = sb("VA", [128, 16, 512], BF16, p1)
                  QAc = sb("QAc", [128, 4, 512], BF16, p1)
                  QIc = sb("QIc", [128, 4, 512], BF16, p1)
                  score4 = [sb("score%d" % i, [128, S], F32, p1) for i in range(4)]
                  score = score4[0]
                  mb = sb("mb", [128, S], BF16, p1)
                  junk = mb
                  maskbT = sb("maskbT", [128, 16, 512], BF16, p1)
                  pT = [sb("pT%d" % i, [128, 512], BF16, p1) for i in range(3)]
                  zs = sb("zs", [128, 512], F32, p1)
                  t1 = sb("t1", [128, 512], F32, p1)
                  Ct = sb("Ct", [128, 512], F32, p1)
                  St = sb("St", [128, 512], F32, p1)
                  posi = sb("posi", [128, 512], I32, p1)
                  posf = sb("posf", [128, 512], F32, p1)
                  ang = sb("ang", [128, 512], F32, p1)
                  uu = zs
                  kk = posi
                  xb = [sb("xb%d" % i, [128, D], BF16, p1) for i in range(1)]
                  rden = sb("rden", [128, 512], F32, p1)
                  relu_t = [sb("relu%d" % i, [128, 512], F32, p1) for i in range(2)]
                  wi_t = sb("wi_t", [128, 8], F32, p1)
                  wabs = sb("wabs", [128, 8], F32, p1)
                  wsgn = sb("wsgn", [128, 8], F32, p1)
                  bs = sb("bs", [128, 24], F32, p1)
                  LO4, W4, MID4, CNT4, GE4 = (bs[:, 4 * i:4 * i + 4] for i in range(5))
                  wring = [sb("wring%d" % i, [128, 8, 256], BF16, p1) for i in range(3)]
                  psG = [psum("psG%d" % i, [128, 512], F32, p1) for i in range(2)]
                  psO = [psum("psO%d" % i, [128, 512], F32, p1) for i in range(2)]
                  psD = [psum("psD%d" % i, [128, 512], F32, p1) for i in range(2)]
                  psT = psum("psT", [128, 1024], BF16, p1)
                  psR = psum("psR", [128, 512], F32, p1)

                  def ld_cols(c0, n, dst0=0):
                      def f(slot):
                          return [(slot[:, :, dst0:dst0 + n], w_in_v[:, :, c0:c0 + n])]
                      return f

                  def ld_ki(slot):
                      return [(slot[:, :, 0:64], w_in_v[:, :, OFF_KI:OFF_KI + 64]),
                              (slot[:, :, 64:128], w_in_v[:, :, OFF_KI:OFF_KI + 64])]

                  loads1 = []
                  for tc in range(4):
                      for off in (OFF_QA, OFF_KA, OFF_QI):
                          loads1 += [ld_cols(off, 256), ld_cols(off + 256, 256)]
                      loads1 += [ld_ki]
                      loads1 += [ld_cols(OFF_VA, 256), ld_cols(OFF_VA + 256, 256)]
                      loads1 += [ld_cols(OFF_WI, 8)]
                  ws1 = Stream(sch, "wring", wring, loads1)

                  gcount = [0]
                  scount = [0]

                  def nextG():
                      i = gcount[0] % 2
                      gcount[0] += 1
                      return psG[i], ("psG", i)

                  for tc in range(0 if KSKIP1 else 4):
                      tok0 = tc * 512
                      for j in range(4):
                          tt = tc * 4 + j
                          xbj = xb[0]
                          sch.dma("pool", xbj[:], x_h.ap()[tt * 128:(tt + 1) * 128, :], key=("xb", 0),
                                  writes=[("xb", 0)])
                          for half in range(2):
                              for k in range(4):
                                  kc = half * 4 + k
                                  sch.pe(lambda t, kc=kc, k=k, xbj=xbj: t.transpose(
                                      out=psT[:, k * 128:(k + 1) * 128], in_=xbj[:, kc * 128:(kc + 1) * 128],
                                      identity=identb[:, :]), reads=[("xb", 0), "identb"], writes=["psT"])
                              sch.act(lambda a, half=half, tt=tt: a.copy(
                                  out=XH[:, half * 4:half * 4 + 4, tt * 128:(tt + 1) * 128],
                                  in_=psT[:, 0:512].rearrange("p (k t) -> p k t", k=4)),
                                  reads=["psT"], writes=[("XH", tc)])
                      build_tables(tc, 0, posi, posf, ang, uu, kk, Ct, St, ukey="zs", kikey="posi")

                      def proj_fm(ncols_chunks, dst_fn, dst_key):
                          for g in range(ncols_chunks // 2):
                              slot, sk = ws1.next()
                              for c2 in range(2):
                                  cc = g * 2 + c2
                                  G, Gk = nextG()
                                  for kc in range(8):
                                      sch.pe(lambda t, G=G, kc=kc, c2=c2, slot=slot: t.matmul(
                                          G[:, :], lhsT=slot[:, kc, c2 * 128:(c2 + 1) * 128],
                                          rhs=XH[:, kc, tok0:tok0 + 512], start=(kc == 0), stop=(kc == 7)),
                                          reads=[sk, ("XH", tc)], writes=[Gk])
                                  rope_block(G, Gk, 0, 128, perm64, "perm64", psR, "psR", zs, t1, Ct, St,
                                             [dst_fn(cc)], [dst_key(cc)])

                      proj_fm(4, lambda cc: QAc[:, cc, :], lambda cc: ("QAc", cc))
                      proj_fm(4, lambda cc: KA[:, cc, tok0:tok0 + 512], lambda cc: ("KA", cc, tc))
                      proj_fm(4, lambda cc: QIc[:, cc, :], lambda cc: ("QIc", cc))
                      slot, sk = ws1.next()
                      G, Gk = nextG()
                      for kc in range(8):
                          sch.pe(lambda t, G=G, kc=kc, slot=slot: t.matmul(
                              G[:, :], lhsT=slot[:, kc, 0:128], rhs=XH[:, kc, tok0:tok0 + 512],
                              start=(kc == 0), stop=(kc == 7)), reads=[sk, ("XH", tc)], writes=[Gk])
                      rope_block(G, Gk, 0, 128, perm64, "perm64", psR, "psR", zs, t1, Ct, St,
                                 [KI2[:, tok0:tok0 + 512]], [("KI2", tc)])
                      for g in range(2):
                          slot, sk = ws1.next()
                          for j in range(4):
                              tt = tc * 4 + j
                              G, Gk = nextG()
                              for kc in range(8):
                                  sch.pe(lambda t, G=G, kc=kc, slot=slot, tt=tt: t.matmul(
                                      G[:, 0:256], lhsT=XH[:, kc, tt * 128:(tt + 1) * 128], rhs=slot[:, kc, 0:256],
                                      start=(kc == 0), stop=(kc == 7)), reads=[sk, ("XH", tc)], writes=[Gk])
                              sch.act(lambda a, G=G, tt=tt, g=g: a.copy(out=VA[:, tt, g * 256:(g + 1) * 256],
                                                                        in_=G[:, 0:256]),
                                      reads=[Gk], writes=[("VA", tt, g)])
                      slot_wi, sk_wi = ws1.next()

                      n_c = (tc + 1) * 512
                      cs = float((64 ** -0.5) * (8 ** -0.5))
                      for j in range(4):
                          tt = tc * 4 + j
                          n_s = (tt + 1) * 128
                          SC = score4[j]
                          SCk = ("score", j)
                          G, Gk = nextG()
                          for kc in range(8):
                              sch.pe(lambda t, G=G, kc=kc, tt=tt: t.matmul(
                                  G[:, 0:8], lhsT=XH[:, kc, tt * 128:(tt + 1) * 128], rhs=slot_wi[:, kc, 0:8],
                                  start=(kc == 0), stop=(kc == 7)), reads=[sk_wi, ("XH", tc)], writes=[Gk])
                          sch.dve(lambda v, G=G: v.tensor_copy(out=wi_t[:, :], in_=G[:, 0:8]), reads=[Gk], writes=["wi_t"])
                          sch.dve(lambda v: v.tensor_scalar(out=wsgn[:, :], in0=wi_t[:, :], scalar1=0.0, scalar2=2.0,
                                                            op0=ALU.is_ge, op1=ALU.mult), reads=["wi_t"], writes=["wsgn"])
                          sch.dve(lambda v: v.tensor_scalar(out=wsgn[:, :], in0=wsgn[:, :], scalar1=-1.0, scalar2=None,
                                                            op0=ALU.add), reads=["wsgn"], writes=["wsgn"])
                          sch.dve(lambda v: v.tensor_tensor(out=wabs[:, :], in0=wi_t[:, :], in1=wsgn[:, :], op=ALU.mult),
                                  reads=["wi_t", "wsgn"], writes=["wabs"])
                          sch.dve(lambda v: v.tensor_scalar(out=wabs[:, :], in0=wabs[:, :], scalar1=cs, scalar2=None,
                                                            op0=ALU.mult), reads=["wabs"], writes=["wabs"])
                          nblk = (n_s + 511) // 512
                          rcount = 0
                          sbanks = [(psG[0], ("psG", 0)), (psG[1], ("psG", 1)), (psO[0], ("psO", 0)), (psO[1], ("psO", 1)),
                                    (psD[0], ("psD", 0)), (psD[1], ("psD", 1))]
                          rbufs = [(relu_t[0], ("relu", 0)), (relu_t[1], ("relu", 1)), (rden, "rden"), (t1, "t1")]
                          for sbk in range(nblk):
                              s0 = sbk * 512
                              w = min(512, n_s - s0)
                              for h in range(8):
                                  hp = (h % 2) * 64
                                  G, Gk = sbanks[scount[0] % len(sbanks)]
                                  scount[0] += 1
                                  sch.pe(lambda t, G=G, h=h, hp=hp, s0=s0, w=w, j=j: t.matmul(
                                      G[:, 0:w], lhsT=QIc[hp:hp + 64, h // 2, j * 128:(j + 1) * 128],
                                      rhs=KI2[hp:hp + 64, s0:s0 + w], start=True, stop=True),
                                      reads=[("QIc", h // 2)] + [("KI2", c) for c in range(tc + 1)], writes=[Gk])
                                  R, Rk = rbufs[scount[0] % len(rbufs)]
                                  sch.act(lambda a, G=G, R=R, h=h, w=w: a.activation(
                                      out=R[:, 0:w], in_=G[:, 0:w], func=AF.Relu, scale=wabs[:, h:h + 1]),
                                      reads=[Gk, "wabs"], writes=[Rk])
                                  if h == 0:
                                      sch.dve(lambda v, R=R, s0=s0, w=w, SC=SC: v.tensor_scalar(
                                          out=SC[:, s0:s0 + w], in0=R[:, 0:w], scalar1=wsgn[:, 0:1], scalar2=None,
                                          op0=ALU.mult), reads=[Rk, "wsgn"], writes=[SCk])
                                  else:
                                      sch.dve(lambda v, R=R, s0=s0, w=w, h=h, SC=SC: v.scalar_tensor_tensor(
                                          out=SC[:, s0:s0 + w], in0=R[:, 0:w], scalar=wsgn[:, h:h + 1],
                                          in1=SC[:, s0:s0 + w], op0=ALU.mult, op1=ALU.add),
                                          reads=[Rk, "wsgn", SCk], writes=[SCk])
                          sch.dve(lambda v, tt=tt, SC=SC: v.tensor_tensor(
                              out=SC[:, tt * 128:(tt + 1) * 128], in0=SC[:, tt * 128:(tt + 1) * 128], in1=tri[:, :],
                              op=ALU.add), reads=[SCk, "tri"], writes=[SCk])
                          if tt == 5:
                              dump("score5", SC[:, 0:n_s], [SCk])
                          if tt >= 2:
                              sch.dve(lambda v, tt=tt, SC=SC, j=j: v.tensor_reduce(out=LO4[:, j:j + 1], in_=SC[:, 0:tt * 128],
                                                                                  axis=AXX, op=ALU.min),
                                      reads=[SCk], writes=[("lo", j)])
                              sch.dve(lambda v, n_s=n_s, SC=SC, j=j: v.tensor_reduce(out=W4[:, j:j + 1], in_=SC[:, 0:n_s],
                                                                                    axis=AXX, op=ALU.max),
                                      reads=[SCk], writes=[("w", j)])
                              sch.dve(lambda v, j=j: v.scalar_tensor_tensor(
                                  out=W4[:, j:j + 1], in0=W4[:, j:j + 1], scalar=1.0, in1=LO4[:, j:j + 1], op0=ALU.add,
                                  op1=ALU.subtract), reads=[("w", j), ("lo", j)], writes=[("w", j)])
                      j0 = 2 if tc == 0 else 0
                      cols = slice(j0, 4)
                      lo_keys = [("lo", j) for j in range(j0, 4)]
                      w_keys = [("w", j) for j in range(j0, 4)]
                      for it in range(N_BISECT):
                          sch.dve(lambda v: v.tensor_scalar(out=W4[:, cols], in0=W4[:, cols], scalar1=0.5, scalar2=None,
                                                            op0=ALU.mult), reads=w_keys, writes=w_keys)
                          sch.dve(lambda v: v.tensor_tensor(out=MID4[:, cols], in0=LO4[:, cols], in1=W4[:, cols], op=ALU.add),
                                  reads=lo_keys + w_keys, writes=["mid"])
                          sch.dve(lambda v: v.memset(CNT4[:, cols], 0.0), reads=["ge"], writes=[("cnt", j) for j in range(j0, 4)])
                          for j in range(j0, 4):
                              n_s = (tc * 4 + j + 1) * 128
                              sch.dve(lambda v, n_s=n_s, j=j: v.tensor_scalar(
                                  out=junk[:, 0:n_s], in0=score4[j][:, 0:n_s], scalar1=MID4[:, j:j + 1], scalar2=0.0,
                                  op0=ALU.is_ge, op1=ALU.add, accum_out=CNT4[:, j:j + 1]),
                                  reads=[("score", j), "mid", ("cnt", j)], writes=[("cnt", j)])
                          sch.dve(lambda v: v.tensor_scalar(out=GE4[:, cols], in0=CNT4[:, cols], scalar1=255.5, scalar2=None,
                                                            op0=ALU.is_ge), reads=[("cnt", j) for j in range(j0, 4)],
                                  writes=["ge"])
                          sch.dve(lambda v: v.tensor_tensor(out=GE4[:, cols], in0=GE4[:, cols], in1=W4[:, cols], op=ALU.mult),
                                  reads=["ge"] + w_keys, writes=["ge"])
                          sch.dve(lambda v: v.tensor_tensor(out=LO4[:, cols], in0=LO4[:, cols], in1=GE4[:, cols], op=ALU.add),
                                  reads=["ge"] + lo_keys, writes=lo_keys)
                      if tc == 0:
                          sch.dve(lambda v: v.memset(LO4[:, 0:2], -20000.0), writes=[("lo", 0), ("lo", 1)])
                      for j in range(4):
                          tt = tc * 4 + j
                          n_s = (tt + 1) * 128
                          sch.dve(lambda v, n_s=n_s, j=j: v.tensor_scalar(
                              out=mb[:, 0:n_s], in0=score4[j][:, 0:n_s], scalar1=LO4[:, j:j + 1], scalar2=NEG, op0=ALU.is_lt,
                              op1=ALU.mult), reads=[("score", j), ("lo", j)], writes=["mb"])
                          if n_c > n_s:
                              sch.dve(lambda v, n_s=n_s: v.memset(mb[:, n_s:n_c], NEG), reads=["mb"], writes=["mb"])
                          for sg in range(tc + 1):
                              for k in range(4):
                                  sch.pe(lambda t, sg=sg, k=k: t.transpose(
                                      out=psT[:, k * 128:(k + 1) * 128],
                                      in_=mb[:, (sg * 4 + k) * 128:(sg * 4 + k + 1) * 128], identity=identb[:, :]),
                                      reads=["mb", "identb"], writes=["psT"])
                              sch.act(lambda a, sg=sg, j=j: a.copy(
                                  out=maskbT[:, sg * 4:sg * 4 + 4, j * 128:(j + 1) * 128],
                                  in_=psT[:, 0:512].rearrange("p (k t) -> p k t", k=4)),
                                  reads=["psT"], writes=[("maskbT", sg)])
                      nst = (tc + 1) * 4
                      heads = []
                      for h in range(8):
                          hp = (h % 2) * 64
                          cc = h // 2
                          heads.append(dict(
                              kT_fn=lambda st, hp=hp, cc=cc: KA[hp:hp + 64, cc, st * 128:(st + 1) * 128],
                              qT_fn=lambda hp=hp, cc=cc: QAc[hp:hp + 64, cc, :],
                              kq_reads=[("KA", cc, c) for c in range(tc + 1)] + [("QAc", cc)],
                              bias_fn=lambda st: (maskbT[:, st, :], [("maskbT", st // 4)]),
                              v_fn=lambda st, cc=cc: VA[:, st, cc * 128:(cc + 1) * 128],
                              v_reads=[("VA", t_, cc // 2) for t_ in range(nst)],
                              rows=(hp, hp + 64),
                              out_ap=oaT[hp:hp + 64, cc, tok0:tok0 + 512], out_key=("oaT", cc, tc, h % 2)))
                      attn_chunk(nst, heads, psG, psO, psD, pT, rden, float(64 ** -0.5))
                  if "oaT" in dbg_out:
                      for cc in range(4):
                          sch.dve(lambda v, cc=cc: v.tensor_copy(out=score[:, :], in_=oaT[:, cc, :]),
                                  reads=[("oaT", cc, c, q) for c in range(4) for q in range(2)], writes=["score"])
                          dump("oaT", score[:, :], ["score"], rows=(cc * 128, (cc + 1) * 128))
                  sch.flush()
                  if stop_after == 1:
                      raise _Stop()

              p2 = ExitStack()
              with p2:
                  Kh = sb("Kh", [128, 8, S], BF16, p2)
                  Vb = sb("Vb", [128, 16, 512], BF16, p2)
                  Qh = sb("Qh", [128, 8, 512], BF16, p2)
                  cqg = sb("cqg", [128, 3, 512], BF16, p2)
                  ckvg = sb("ckvg", [128, 2, 512], BF16, p2)
                  sq = sb("sq", [128, 512], F32, p2)
                  sqv = sb("sqv", [128, 2, 512], F32, p2)
                  rq_bc = sb("rq_bc", [128, 512], F32, p2)
                  rkv_bc = sb("rkv_bc", [128, 512], F32, p2)
                  rkv_tok = sb("rkv_tok", [128, 4], F32, p2)
                  pT = [sb("pT2_%d" % i, [128, 512], BF16, p2) for i in range(3)]
                  zs = sb("zs2", [128, 512], F32, p2)
                  t1 = sb("t1_2", [128, 512], F32, p2)
                  Ct = sb("Ct2", [128, 512], F32, p2)
                  St = sb("St2", [128, 512], F32, p2)
                  posi = sb("posi2", [128, 512], I32, p2)
                  posf = sb("posf2", [128, 512], F32, p2)
                  ang = sb("ang2", [128, 512], F32, p2)
                  uu = sb("uu2", [128, 512], F32, p2)
                  kk = sb("kk2", [128, 512], I32, p2)
                  rden = sb("rden2", [128, 512], F32, p2)
                  wuq = sb("wuq", [128, 3, 768], BF16, p2)
                  wukvk = sb("wukvk", [128, 2, 512], BF16, p2)
                  wukvv = sb("wukvv", [128, 2, 512], BF16, p2)
                  gq = sb("gq", [128, 3], F32, p2)
                  gkv = sb("gkv", [128, 2], F32, p2)
                  wring = [sb("wring2_%d" % i, [128, 8, 256], BF16, p2) for i in range(3)]
                  psG = [psum("psG2_%d" % i, [128, 512], F32, p2) for i in range(2)]
                  psO = [psum("psO2_%d" % i, [128, 512], F32, p2) for i in range(2)]
                  psD = [psum("psD2_%d" % i, [128, 512], F32, p2) for i in range(2)]
                  psS = psum("psS2", [128, 512], F32, p2)
                  psR = psum("psR2", [128, 512], F32, p2)

                  sch.dve(lambda v: v.memset(zs[:, :], 0.0), writes=["zs"])
                  sch.dma("pool", wuq[:], wuq_h.ap().rearrange("(kc p) n -> p kc n", p=128), key="c0", writes=["wuq"])
                  sch.dma("pool", wukvk[:], wukv_k_h.ap().rearrange("(kc p) n -> p kc n", p=128), key="c0", writes=["wukvk"])
                  sch.dma("pool", wukvv[:], wukv_v_h.ap().rearrange("(kc p) n -> p kc n", p=128), key="c0", writes=["wukvv"])
                  sch.dma("sp", gq[:], rmscq_h.ap(), key="c1", writes=["gq"])
                  sch.dma("sp", gkv[:], rmsckv_h.ap(), key="c1", writes=["gkv"])

                  def ld_cols(c0, n, dst0=0):
                      def f(slot):
                          return [(slot[:, :, dst0:dst0 + n], w_in_v[:, :, c0:c0 + n])]
                      return f

                  loads2 = []
                  for tc in range(4):
                      loads2 += [ld_cols(OFF_CQ, 256), ld_cols(OFF_CQ + 256, 128), ld_cols(OFF_CKV, 256),
                                 ld_cols(OFF_KR, 32, dst0=64)]
                  ws2 = Stream(sch, "wring2", wring, loads2)
                  gcount = [0]

                  def nextG():
                      i = gcount[0] % 2
                      gcount[0] += 1
                      return psG[i], ("psG", i)

                  for tc in range(4):
                      tok0 = tc * 512
                      build_tables(tc, 1, posi, posf, ang, uu, kk, Ct, St)

                      def latent(nchunks, slots_cols, dst, dstname, gain, gain_key, nfeat, rbc, rbc_key, keep_sq, extra=None):
                          cidx = 0
                          for (ncol_chunks) in slots_cols:
                              slot, sk = ws2.next()
                              for c2 in range(ncol_chunks):
                                  G, Gk = nextG()
                                  for kc in range(8):
                                      sch.pe(lambda t, G=G, kc=kc, c2=c2, slot=slot: t.matmul(
                                          G[:, :], lhsT=slot[:, kc, c2 * 128:(c2 + 1) * 128],
                                          rhs=XH[:, kc, tok0:tok0 + 512], start=(kc == 0), stop=(kc == 7)),
                                          reads=[sk, ("XH", tc)], writes=[Gk])
                                  sqt = sqv[:, cidx, :] if keep_sq else sq[:, :]
                                  sqk = ("sqv", cidx) if keep_sq else "sq"
                                  if KLAT >= 1:
                                      sch.act(lambda a, G=G, sqt=sqt: a.activation(out=sqt, in_=G[:, :], func=AF.Square),
                                              reads=[Gk], writes=[sqk])
                                  if KLAT >= 2:
                                      sch.dve(lambda v, G=G, cidx=cidx: v.tensor_scalar(
                                          out=dst[:, cidx, :], in0=G[:, :], scalar1=gain[:, cidx:cidx + 1], scalar2=None,
                                          op0=ALU.mult), reads=[Gk, gain_key, sqk], writes=[(dstname, cidx)])
                                  if KLAT >= 3:
                                      sch.pe(lambda t, sqt=sqt, cidx=cidx: t.matmul(
                                          psS[:, :], lhsT=onesf[:, :], rhs=sqt, start=(cidx == 0), stop=(cidx == nchunks - 1)),
                                          reads=[sqk, "onesf"], writes=["psS"])
                                  cidx += 1
                              if extra is not None:
                                  extra(slot, sk)
                          if KLAT < 4:
                              return
                          sch.act(lambda a: a.activation(out=rbc[:, :], in_=psS[:, :], func=AF.Sqrt, bias=epsrms[:, 0:1],
                                                         scale=1.0 / nfeat), reads=["psS", "epsrms"], writes=[rbc_key])
                          sch.dve(lambda v: v.reciprocal(out=rbc[:, :], in_=rbc[:, :]), reads=[rbc_key], writes=[rbc_key])

                      if KSUB < 2:
                          continue
                      latent(3, [2, 1], cqg, "cqg", gq, "gq", 384.0, rq_bc, "rq_bc", False)
                      if KSUB < 3:
                          continue
                      def ckv_tok(slot, sk):
                          for j in range(4):
                              tt = tc * 4 + j
                              G, Gk = nextG()
                              for kc in range(8):
                                  sch.pe(lambda t, G=G, kc=kc, tt=tt, slot=slot: t.matmul(
                                      G[:, 0:256], lhsT=XH[:, kc, tt * 128:(tt + 1) * 128], rhs=slot[:, kc, 0:256],
                                      start=(kc == 0), stop=(kc == 7)), reads=[sk, ("XH", tc)], writes=[Gk])
                              sch.dve(lambda v, G=G: v.tensor_copy(out=sq[:, 0:256], in_=G[:, 0:256]), reads=[Gk], writes=["sq"])
                              sch.dve(lambda v: v.tensor_tensor(out=sq[:, 0:256], in0=sq[:, 0:256], in1=sq[:, 0:256],
                                                                op=ALU.mult), reads=["sq"], writes=["sq"])
                              sch.dve(lambda v, j=j: v.tensor_reduce(out=rkv_tok[:, j:j + 1], in_=sq[:, 0:256], axis=AXX,
                                                                     op=ALU.add), reads=["sq"], writes=["rkv_tok"])
                          sch.act(lambda a: a.activation(out=rkv_tok[:, :], in_=rkv_tok[:, :], func=AF.Sqrt,
                                                         bias=epsrms[:, 0:1], scale=1.0 / 256.0),
                                  reads=["rkv_tok", "epsrms"], writes=["rkv_tok"])

                      latent(2, [2], ckvg, "ckvg", gkv, "gkv", 256.0, rkv_bc, "rkv_bc", False, extra=ckv_tok)
                      sch.dve(lambda v: v.reciprocal(out=rkv_tok[:, :], in_=rkv_tok[:, :]), reads=["rkv_tok"],
                              writes=["rkv_tok"])
                      if KSUB < 4:
                          continue
                      slot, sk = ws2.next()
                      G, Gk = nextG()
                      for kc in range(8):
                          sch.pe(lambda t, G=G, kc=kc, slot=slot: t.matmul(
                              G[0:96, :], lhsT=slot[:, kc, 0:96], rhs=XH[:, kc, tok0:tok0 + 512],
                              start=(kc == 0), stop=(kc == 7)), reads=[sk, ("XH", tc)], writes=[Gk])
                      rope_block(G, Gk, 64, 96, perm16, "perm16", psR, "psR", zs, t1, Ct, St,
                                 [Kh[64:96, h, tok0:tok0 + 512] for h in range(8)], [("Kh_r", h, tc) for h in range(8)])
                      if KSUB < 5:
                          continue
                      for h in range(8):
                          G, Gk = nextG()
                          for kc in range(3):
                              sch.pe(lambda t, G=G, kc=kc, h=h: t.matmul(
                                  G[0:96, :], lhsT=wuq[:, kc, h * 96:(h + 1) * 96], rhs=cqg[:, kc, :],
                                  start=(kc == 0), stop=(kc == 2)), reads=["wuq"] + [("cqg", c) for c in range(3)],
                                  writes=[Gk])
                          sch.dve(lambda v, G=G, h=h: v.tensor_tensor(out=Qh[0:64, h, :], in0=G[0:64, :], in1=rq_bc[0:64, :],
                                                                      op=ALU.mult), reads=[Gk, "rq_bc"], writes=[("Qh_n", h)])
                          sch.dve(lambda v, G=G: v.tensor_tensor(out=zs[64:96, :], in0=G[64:96, :], in1=rq_bc[64:96, :],
                                                                 op=ALU.mult), reads=[Gk, "rq_bc"], writes=["zs"])
                          sch.pe(lambda t: t.matmul(psR[:, :], lhsT=perm16[:, :], rhs=zs[:, :], start=True,
                                                    stop=True), reads=["zs", "perm16"], writes=["psR"])
                          sch.dve(lambda v: v.tensor_tensor(out=t1[64:96, :], in0=zs[64:96, :], in1=Ct[64:96, :], op=ALU.mult),
                                  reads=["zs", ("tab", "C")], writes=["t1"])
                          sch.dve(lambda v: v.tensor_tensor(out=zs[64:96, :], in0=psR[64:96, :], in1=St[64:96, :], op=ALU.mult),
                                  reads=["psR", ("tab", "S")], writes=["zs"])
                          sch.dve(lambda v, h=h: v.tensor_tensor(out=Qh[64:96, h, :], in0=t1[64:96, :], in1=zs[64:96, :],
                                                                 op=ALU.add), reads=["t1", "zs"], writes=[("Qh_r", h)])
                          G, Gk = nextG()
                          for kc in range(2):
                              sch.pe(lambda t, G=G, kc=kc, h=h: t.matmul(
                                  G[0:64, :], lhsT=wukvk[:, kc, h * 64:(h + 1) * 64], rhs=ckvg[:, kc, :],
                                  start=(kc == 0), stop=(kc == 1)), reads=["wukvk", ("ckvg", 0), ("ckvg", 1)], writes=[Gk])
                          sch.dve(lambda v, G=G, h=h: v.tensor_tensor(
                              out=Kh[0:64, h, tok0:tok0 + 512], in0=G[0:64, :], in1=rkv_bc[0:64, :], op=ALU.mult),
                              reads=[Gk, "rkv_bc"], writes=[("Kh_n", h, tc)])
                      if KSUB < 6:
                          continue
                      for j in range(4):
                          tt = tc * 4 + j
                          G, Gk = nextG()
                          for kc in range(2):
                              sch.pe(lambda t, G=G, kc=kc, j=j: t.matmul(
                                  G[:, :], lhsT=ckvg[:, kc, j * 128:(j + 1) * 128], rhs=wukvv[:, kc, :],
                                  start=(kc == 0), stop=(kc == 1)), reads=["wukvv", ("ckvg", 0), ("ckvg", 1)], writes=[Gk])
                          sch.dve(lambda v, G=G, tt=tt, j=j: v.tensor_scalar(
                              out=Vb[:, tt, :], in0=G[:, :], scalar1=rkv_tok[:, j:j + 1], scalar2=None, op0=ALU.mult),
                              reads=[Gk, "rkv_tok"], writes=[("Vb", tt)])
                      if KSUB < 7:
                          continue
                      nst = (tc + 1) * 4
                      heads = []
                      for h in range(8):
                          hp = (h % 2) * 64
                          cc = h // 2
                          heads.append(dict(
                              kT_fn=lambda st, h=h: Kh[0:96, h, st * 128:(st + 1) * 128],
                              qT_fn=lambda h=h: Qh[0:96, h, :],
                              kq_reads=[("Kh_n", h, c) for c in range(tc + 1)] + [("Kh_r", h, c) for c in range(tc + 1)] +
                                       [("Qh_n", h), ("Qh_r", h)],
                              bias_fn=lambda st, tc=tc: ((triT[:, st - tc * 4, :], ["triT"]) if st >= tc * 4 else None),
                              v_fn=lambda st, cc=cc: Vb[:, st, cc * 128:(cc + 1) * 128],
                              v_reads=[("Vb", t_) for t_ in range(nst)],
                              rows=(hp, hp + 64),
                              out_ap=obT[hp:hp + 64, cc, tok0:tok0 + 512], out_key=("obT", cc, tc, h % 2)))
                      attn_chunk(nst, heads, psG, psO, psD, pT, rden, float(96 ** -0.5))
                  if "obT" in dbg_out:
                      for cc in range(4):
                          sch.dve(lambda v, cc=cc: v.tensor_copy(out=sqv[:, :, :].rearrange("p a b -> p (a b)"),
                                                                 in_=obT[:, cc, 0:1024]),
                                  reads=[("obT", cc, c, q) for c in range(4) for q in range(2)], writes=["sqv"])
                          dump("obT", sqv[:, :, :].rearrange("p a b -> p (a b)"), ["sqv"], rows=(cc * 128, (cc + 1) * 128))
                  sch.flush()
                  if stop_after == 2:
                      raise _Stop()

              p3 = ExitStack()
              with p3:
                  gT = sb("gT", [128, 16, 512], BF16, p3)
                  mixT = sb("mixT", [128, 8, 512], BF16, p3)
                  m0 = sb("m0", [128, 512], F32, p3)
                  mm0 = [m0, sb("m0b", [128, 512], F32, p3)]
                  m1 = sb("m1", [128, 512], F32, p3)
                  bgate = sb("bgate", [128, 16], F32, p3)
                  xt = [sb("xt%d" % i, [128, D], F32, p3) for i in range(2)]
                  hpre = sb("hpre", [128, D], F32, p3)
                  ustage = [sb("ustage%d" % i, [128, D], F32, p3) for i in range(4)]
                  hh = [sb("hh%d" % i, [128, D], F32, p3) for i in range(2)]
                  junk = sb("junk3", [128, D], F32, p3)
                  g1bc = sb("g1bc", [128, D], F32, p3)
                  b1bc = sb("b1bc", [128, D], F32, p3)
                  st_ = sb("st3", [128, 8], F32, p3)
                  hTf = sb("hTf", [128, 8, 128], F32, p3)
                  wrt = sb("wrt", [128, 8, E], F32, p3)
                  brbc = sb("brbc", [128, E], F32, p3)
                  lg = sb("lg", [128, E], F32, p3)
                  m8 = sb("m8", [128, 8], F32, p3)
                  ex = sb("ex", [128, E], F32, p3)
                  msk = sb("msk", [128, E], F32, p3)
                  wring = [sb("wring3_%d" % i, [128, 8, 256], BF16, p3) for i in range(3)]
                  psG = [psum("psG3_%d" % i, [128, 512], F32, p3) for i in range(4)]
                  psU = [psum("psU3_%d" % i, [128, 512], F32, p3) for i in range(2)]
                  psTf = psum("psTf", [128, 512], F32, p3)
                  psL3 = psum("psL3", [128, 512], F32, p3)

                  sch.dma("sp", bgate[:], bgate_h.ap(), key="c1", writes=["bgate"])
                  sch.dma("sp", g1bc[:], bass.AP(ln1g_h, 0, [[0, 128], [1, D]]), key="c1", writes=["g1bc"])
                  sch.dma("sp", b1bc[:], bass.AP(ln1b_h, 0, [[0, 128], [1, D]]), key="c1", writes=["b1bc"])
                  sch.dma("sp", wrt[:], wr_h.ap().rearrange("(kc p) n -> p kc n", p=128), key="c1", writes=["wrt"])
                  sch.dma("sp", brbc[:], bass.AP(br_h, 0, [[0, 128], [1, E]]), key="c1", writes=["brbc"])

                  woa_v = woa_h.ap().rearrange("(kc p) n -> p kc n", p=128)
                  wob_v = wob_h.ap().rearrange("(kc p) n -> p kc n", p=128)
                  wout_v = wout_h.ap().rearrange("(kc p) n -> p kc n", p=128)

                  def ld3(view, nk, c0):
                      def f(slot):
                          return [(slot[:, 0:nk, :], view[:, :, c0:c0 + 256])]
                      return f

                  loads3 = []
                  for tc in range(4):
                      for g in range(4):
                          loads3 += [ld3(w_in_v, 8, OFF_G + g * 256), ld3(w_in_v, 8, OFF_G + 1024 + g * 256),
                                     ld3(woa_v, 4, g * 256), ld3(wob_v, 4, g * 256)]
                      for g in range(4):
                          loads3 += [ld3(wout_v, 8, g * 256)]
                  ws3 = Stream(sch, "wring3", wring, loads3)
                  gcount = [0]

                  def nextG():
                      i = gcount[0] % 4
                      gcount[0] += 1
                      return psG[i], ("psG", i)

                  for tc in range(4):
                      tok0 = tc * 512
                      for g in range(4):
                          for gi in range(2):
                              slot, sk = ws3.next()
                              for c2 in range(2):
                                  fc = g * 2 + c2
                                  G, Gk = nextG()
                                  for kc in range(8):
                                      sch.pe(lambda t, G=G, kc=kc, c2=c2, slot=slot: t.matmul(
                                          G[:, :], lhsT=slot[:, kc, c2 * 128:(c2 + 1) * 128], rhs=XH[:, kc, tok0:tok0 + 512],
                                          start=(kc == 0), stop=(kc == 7)), reads=[sk, ("XH", tc)], writes=[Gk])
                                  col = gi * 8 + fc
                                  sch.act(lambda a, G=G, col=col: a.activation(
                                      out=gT[:, col, :], in_=G[:, :], func=AF.Sigmoid, bias=bgate[:, col:col + 1], scale=1.0),
                                      reads=[Gk, "bgate"], writes=[("gT", col)])
                          for (src, srck, gi) in ((oaT, "oaT", 0), (obT, "obT", 1)):
                              slot, sk = ws3.next()
                              for c2 in range(2):
                                  fc = g * 2 + c2
                                  G, Gk = nextG()
                                  for kc in range(4):
                                      sch.pe(lambda t, G=G, kc=kc, c2=c2, slot=slot, src=src: t.matmul(
                                          G[:, :], lhsT=slot[:, kc, c2 * 128:(c2 + 1) * 128], rhs=src[:, kc, tok0:tok0 + 512],
                                          start=(kc == 0), stop=(kc == 3)), reads=[sk, srck], writes=[Gk])
                                  col = gi * 8 + fc
                                  if gi == 0:
                                      sch.dve(lambda v, G=G, col=col, c2=c2: v.tensor_tensor(
                                          out=mm0[c2][:, :], in0=G[:, :], in1=gT[:, col, :], op=ALU.mult),
                                          reads=[Gk, ("gT", col)], writes=[("mm0", c2)])
                                  else:
                                      sch.dve(lambda v, G=G, col=col: v.tensor_tensor(
                                          out=m1[:, :], in0=G[:, :], in1=gT[:, col, :], op=ALU.mult),
                                          reads=[Gk, ("gT", col)], writes=["m1"])
                                      sch.dve(lambda v, fc=fc, c2=c2: v.tensor_tensor(
                                          out=mixT[:, fc, :], in0=mm0[c2][:, :], in1=m1[:, :], op=ALU.add),
                                          reads=[("mm0", c2), "m1"], writes=[("mixT", fc)])
                      if tc == 0 and "mixT0" in dbg_out:
                          for fc in range(8):
                              sch.dve(lambda v, fc=fc: v.tensor_copy(out=m0[:, :], in_=mixT[:, fc, :]),
                                      reads=[("mixT", fc), ("mm0", 0)], writes=[("mm0", 0)])
                              dump("mixT0", m0[:, :], [("mm0", 0)], rows=(fc * 128, (fc + 1) * 128))
                          for col in range(16):
                              sch.dve(lambda v, col=col: v.tensor_copy(out=m1[:, :], in_=gT[:, col, :]),
                                      reads=[("gT", col), "m1"], writes=["m1"])
                              dump("gT0", m1[:, :], ["m1"], rows=(col * 128, (col + 1) * 128))
                      for g in range(4):
                          slot, sk = ws3.next()
                          for j in range(4):
                              U = psU[(g * 4 + j) % 2]
                              Uk = ("psU", (g * 4 + j) % 2)
                              for fc in range(8):
                                  sch.pe(lambda t, U=U, fc=fc, j=j, slot=slot: t.matmul(
                                      U[:, 0:256], lhsT=mixT[:, fc, j * 128:(j + 1) * 128], rhs=slot[:, fc, :],
                                      start=(fc == 0), stop=(fc == 7)), reads=[sk] + [("mixT", f) for f in range(8)],
                                      writes=[Uk])
                              sch.act(lambda a, U=U, g=g, j=j: a.copy(out=ustage[j][:, g * 256:(g + 1) * 256], in_=U[:, 0:256]),
                                      reads=[Uk], writes=[("ustage", j, g)])
                      for j in range(4):
                          tt = tc * 4 + j
                          X = xt[tt % 2]
                          Xk = ("xt", tt % 2)
                          H = hh[tt % 2]
                          Hk = ("hh", tt % 2)
                          sch.dma("sp", X[:], x_h.ap()[tt * 128:(tt + 1) * 128, :], key=("xt", tt % 2), writes=[Xk])
                          sch.dve(lambda v, X=X, j=j: v.scalar_tensor_tensor(
                              out=hpre[:, :], in0=X[:, :], scalar=DN_ALPHA, in1=ustage[j][:, :], op0=ALU.mult, op1=ALU.add),
                              reads=[Xk] + [("ustage", j, g) for g in range(4)], writes=["hpre"])
                          layer_norm(sch, hpre, "hpre", H, Hk, junk, "junk", st_, "st", g1bc, "g1bc", b1bc, "b1bc", epsln)
                          if tt == 0:
                              dump("h0", H[:, :], [Hk])
                          sch.dma("sp", hs_h.ap()[tt * 128:(tt + 1) * 128, :], H[:, :], key=("hst", tt % 2), reads=[Hk])
                          for half in range(2):
                              for k in range(4):
                                  kc = half * 4 + k
                                  sch.pe(lambda t, H=H, kc=kc, k=k: t.transpose(
                                      out=psTf[:, k * 128:(k + 1) * 128], in_=H[:, kc * 128:(kc + 1) * 128],
                                      identity=identf[:, :]), reads=[Hk, "identf"], writes=["psTf"])
                              sch.act(lambda a, half=half: a.copy(
                                  out=hTf[:, half * 4:half * 4 + 4, :], in_=psTf[:, :].rearrange("p (k t) -> p k t", k=4)),
                                  reads=["psTf"], writes=[("hTf", half)])
                              sch.dve(lambda v, half=half, tt=tt: v.tensor_copy(
                                  out=XH[:, half * 4:half * 4 + 4, tt * 128:(tt + 1) * 128],
                                  in_=psTf[:, :].rearrange("p (k t) -> p k t", k=4)),
                                  reads=["psTf", ("hTf", half)], writes=[("XH", tc)])
                          for kc in range(8):
                              sch.pe(lambda t, kc=kc: t.matmul(psL3[:, 0:E], lhsT=hTf[:, kc, :], rhs=wrt[:, kc, :],
                                                               start=(kc == 0), stop=(kc == 7)),
                                     reads=[("hTf", 0), ("hTf", 1), "wrt"], writes=["psL3"])
                          sch.dve(lambda v: v.tensor_tensor(out=lg[:, :], in0=psL3[:, 0:E], in1=brbc[:, :], op=ALU.add),
                                  reads=["psL3", "brbc"], writes=["lg"])
                          sch.dve(lambda v: v.max(out=m8[:, :], in_=lg[:, :]), reads=["lg"], writes=["m8"])
                          sch.dve(lambda v: v.tensor_scalar(out=msk[:, :], in0=lg[:, :], scalar1=m8[:, 3:4], scalar2=None,
                                                            op0=ALU.is_ge), reads=["lg", "m8"], writes=["msk"])
                          sch.dve(lambda v: v.tensor_scalar(out=ex[:, :], in0=lg[:, :], scalar1=m8[:, 0:1], scalar2=None,
                                                            op0=ALU.subtract), reads=["lg", "m8"], writes=["ex"])
                          sch.act(lambda a: a.activation(out=ex[:, :], in_=ex[:, :], func=AF.Exp), reads=["ex"], writes=["ex"])
                          sch.dve(lambda v: v.tensor_tensor(out=ex[:, :], in0=ex[:, :], in1=msk[:, :], op=ALU.mult),
                                  reads=["ex", "msk"], writes=["ex"])
                          sch.dve(lambda v: v.tensor_reduce(out=m8[:, 4:5], in_=ex[:, :], axis=AXX, op=ALU.add),
                                  reads=["ex"], writes=["m8"])
                          sch.dve(lambda v: v.reciprocal(out=m8[:, 5:6], in_=m8[:, 4:5]), reads=["m8"], writes=["m8"])
                          sch.dve(lambda v, tt=tt: v.tensor_scalar(out=comb[:, tt, :], in0=ex[:, :], scalar1=m8[:, 5:6],
                                                                   scalar2=None, op0=ALU.mult), reads=["ex", "m8"],
                                  writes=[("comb", tt)])
                  if "comb" in dbg_out:
                      dump("comb", comb[:, :, :].rearrange("p a b -> p (a b)"), [("comb", t_) for t_ in range(16)])
                  sch.flush()
                  if stop_after == 3:
                      raise _Stop()
          p4 = ExitStack()
          with p4:
              acc = sb("acc", [128, 16, D], F32, p4)
              hid = sb("hid", [128, 8, S], BF16, p4)
              bup = sb("bup", [128, E * 16], F32, p4)
              bdn = sb("bdn", [E, D], F32, p4)
              combT = sb("combT", [E, 128], F32, p4)
              gt = [sb("gt%d" % i, [128, 512], F32, p4) for i in range(2)]
              sg = [sb("sg%d" % i, [128, 512], F32, p4) for i in range(2)]
              lt = [sb("lt%d" % i, [128, 512], F32, p4) for i in range(2)]
              g2bc = sb("g2bc", [128, D], F32, p4)
              b2bc = sb("b2bc", [128, D], F32, p4)
              junk = sb("junk4", [128, D], F32, p4)
              yo = [sb("yo%d" % i, [128, D], F32, p4) for i in range(2)]
              st_ = sb("st4", [128, 8], F32, p4)
              wring = [sb("wring4_%d" % i, [128, 8, 512], BF16, p4) for i in range(3)]
              psA = [psum("psA%d" % i, [128, 512], F32, p4) for i in range(4)]
              psY = [psum("psY%d" % i, [128, 512], F32, p4) for i in range(2)]
              psC = psum("psC", [128, 512], F32, p4)

              sch.dma("sp", bup[:], bup_h.ap(), key="c1", writes=["bup"])
              sch.dma("sp", bdn[:], bdn_h.ap(), key="c1", writes=["bdn"])
              sch.dma("sp", g2bc[:], bass.AP(ln2g_h, 0, [[0, 128], [1, D]]), key="c1", writes=["g2bc"])
              sch.dma("sp", b2bc[:], bass.AP(ln2b_h, 0, [[0, 128], [1, D]]), key="c1", writes=["b2bc"])
              for tt in range(16):
                  Y = yo[tt % 2]
                  Yk = ("yo", tt % 2)
                  sch.dma("sp", Y[:], hs_h.ap()[tt * 128:(tt + 1) * 128, :], key=("yo", tt % 2), writes=[Yk])
                  sch.pe(lambda t, tt=tt: t.transpose(out=psC[0:E, 0:128], in_=comb[:, tt, :], identity=identf[:, :]),
                         reads=[("comb", tt), "identf"], writes=["psC"])
                  sch.act(lambda a: a.copy(out=combT[:, :], in_=psC[0:E, 0:128]), reads=["psC"], writes=["combT"])
                  for half in range(2):
                      Yp = psY[half]
                      sch.pe(lambda t, Yp=Yp, half=half: t.matmul(Yp[:, :], lhsT=combT[:, :],
                                                                  rhs=bdn[:, half * 512:(half + 1) * 512], start=True,
                                                                  stop=True), reads=["combT", "bdn"], writes=[("psY", half)])
                      sch.dve(lambda v, Yp=Yp, half=half, tt=tt, Y=Y: v.scalar_tensor_tensor(
                          out=acc[:, tt, half * 512:(half + 1) * 512], in0=Y[:, half * 512:(half + 1) * 512], scalar=DN_ALPHA,
                          in1=Yp[:, :], op0=ALU.mult, op1=ALU.add), reads=[Yk, ("psY", half)], writes=[("acc", tt, half)])

              wup_v = [wup_h.ap()[e].rearrange("(kc p) n -> p kc n", p=128) for e in range(E)]
              wdn_v = [wdn_h.ap()[e].rearrange("(kc p) n -> p kc n", p=128) for e in range(E)]

              def ld_up(e, s):
                  def f(slot):
                      return [(slot[:, :, 0:256], wup_v[e][:, :, s * 256:(s + 1) * 256]),
                              (slot[:, :, 256:512], wup_v[e][:, :, DFF + s * 256:DFF + (s + 1) * 256])]
                  return f

              def ld_dn(e, half):
                  def f(slot):
                      return [(slot[:, :, :], wdn_v[e][:, :, half * 512:(half + 1) * 512])]
                  return f

              loads4 = []
              for e in range(E):
                  loads4 += [ld_up(e, s) for s in range(4)] + [ld_dn(e, hf) for hf in range(2)]
              ws4 = Stream(sch, "wring4", wring, loads4)
              acount = [0]

              def nextA():
                  i = acount[0] % 4
                  acount[0] += 1
                  return psA[i], ("psA", i)

              ycount = [0]
              tcount = [0]
              for e in range(E):
                  for s in range(4):
                      slot, sk = ws4.next()
                      for tb in range(4):
                          tok0 = tb * 512
                          for c2 in range(2):
                              jf = s * 2 + c2
                              Ag, Agk = nextA()
                              Al, Alk = nextA()
                              for (A, Ak, coff) in ((Ag, Agk, 0), (Al, Alk, 256)):
                                  for kc in range(8):
                                      sch.pe(lambda t, A=A, kc=kc, c2=c2, coff=coff, slot=slot: t.matmul(
                                          A[:, :], lhsT=slot[:, kc, coff + c2 * 128:coff + (c2 + 1) * 128],
                                          rhs=XH[:, kc, tok0:tok0 + 512], start=(kc == 0), stop=(kc == 7)),
                                          reads=[sk] + [("XH", tb)], writes=[Ak])
                              i = tcount[0] % 2
                              tcount[0] += 1
                              GT, SG, LT = gt[i], sg[i], lt[i]
                              bg = bup[:, e * 16 + jf:e * 16 + jf + 1]
                              bl = bup[:, e * 16 + 8 + jf:e * 16 + 8 + jf + 1]
                              sch.dve(lambda v, Ag=Ag, GT=GT, bg=bg: v.tensor_scalar(
                                  out=GT[:, :], in0=Ag[:, :], scalar1=bg, scalar2=7.0, op0=ALU.add, op1=ALU.min),
                                  reads=[Agk, "bup"], writes=[("gt", i)])
                              sch.act(lambda a, GT=GT, SG=SG: a.activation(out=SG[:, :], in_=GT[:, :], func=AF.Sigmoid,
                                                                           scale=1.702), reads=[("gt", i)], writes=[("sg", i)])
                              sch.dve(lambda v, Al=Al, LT=LT, bl=bl: v.tensor_scalar(
                                  out=LT[:, :], in0=Al[:, :], scalar1=bl, scalar2=7.0, op0=ALU.add, op1=ALU.min),
                                  reads=[Alk, "bup"], writes=[("lt", i)])
                              sch.dve(lambda g_, LT=LT: g_.tensor_scalar(
                                  out=LT[:, :], in0=LT[:, :], scalar1=-7.0, scalar2=1.0, op0=ALU.max, op1=ALU.add),
                                  reads=[("lt", i)], writes=[("lt", i)])
                              sch.dve(lambda g_, GT=GT, SG=SG: g_.tensor_tensor(out=GT[:, :], in0=GT[:, :], in1=SG[:, :],
                                                                               op=ALU.mult),
                                     reads=[("gt", i), ("sg", i)], writes=[("gt", i)])
                              sch.dve(lambda g_, GT=GT, LT=LT, jf=jf, tok0=tok0: g_.tensor_tensor(
                                  out=hid[:, jf, tok0:tok0 + 512], in0=GT[:, :], in1=LT[:, :], op=ALU.mult),
                                  reads=[("gt", i), ("lt", i)], writes=[("hid", jf, tb)])
                  for half in range(2):
                      slot, sk = ws4.next()
                      for tt in range(16):
                          yi = ycount[0] % 2
                          ycount[0] += 1
                          Yp = psY[yi]
                          for jf in range(8):
                              sch.pe(lambda t, Yp=Yp, jf=jf, tt=tt, slot=slot: t.matmul(
                                  Yp[:, :], lhsT=hid[:, jf, tt * 128:(tt + 1) * 128], rhs=slot[:, jf, :],
                                  start=(jf == 0), stop=(jf == 7)), reads=[sk] + [("hid", f, tt // 4) for f in range(8)],
                                  writes=[("psY", yi)])
                          sch.dve(lambda v, Yp=Yp, tt=tt, half=half, e=e: v.scalar_tensor_tensor(
                              out=acc[:, tt, half * 512:(half + 1) * 512], in0=Yp[:, :], scalar=comb[:, tt, e:e + 1],
                              in1=acc[:, tt, half * 512:(half + 1) * 512], op0=ALU.mult, op1=ALU.add),
                              reads=[("psY", yi), ("comb", tt), ("acc", tt, half)], writes=[("acc", tt, half)])
                          if e == E - 1 and half == 1:
                              Y = yo[tt % 2]
                              Yk = ("yo", tt % 2)
                              layer_norm(sch, acc[:, tt, :], [("acc", tt, 0), ("acc", tt, 1)], Y, Yk, junk, "junk", st_, "st",
                                         g2bc, "g2bc", b2bc, "b2bc", epsln, in_is_ap=True)
                              sch.dma("sp", y_h.ap()[tt * 128:(tt + 1) * 128, :], Y[:, :], key=("yst", tt % 2), reads=[Yk])
              sch.flush()
    except _Stop:
        pass
    return nc


def layer_norm(sch, src, src_key, dst, dst_key, junk, junk_key, st, st_key, gbc, gk, bbc, bk, eps, in_is_ap=False):
    s = src if in_is_ap else src[:, :]
    sk = src_key if isinstance(src_key, list) else [src_key]
    MEAN, SS, RSTD, NM = (st[:, i:i + 1] for i in range(4))
    sch.dve(lambda v: v.tensor_reduce(out=MEAN, in_=s, axis=AXX, op=ALU.add), reads=sk, writes=[st_key])
    sch.dve(lambda v: v.tensor_scalar(out=NM, in0=MEAN, scalar1=-1.0 / D, scalar2=None, op0=ALU.mult),
            reads=[st_key], writes=[st_key])
    sch.dve(lambda v: v.tensor_scalar(out=junk[:, :], in0=s, scalar1=NM, scalar2=None, op0=ALU.add),
            reads=sk + [st_key], writes=[junk_key])
    sch.dve(lambda v: v.memset(SS, 0.0), reads=[st_key], writes=[st_key])
    sch.act(lambda a: a.activation(out=dst[:, :], in_=junk[:, :], func=AF.Square, accum_out=SS),
            reads=[junk_key, st_key], writes=[dst_key, st_key])
    sch.act(lambda a: a.activation(out=RSTD, in_=SS, func=AF.Sqrt, bias=eps[:, 0:1], scale=1.0 / D),
            reads=[st_key, "epsln"], writes=[st_key])
    sch.dve(lambda v: v.reciprocal(out=RSTD, in_=RSTD), reads=[st_key], writes=[st_key])
    sch.dve(lambda v: v.scalar_tensor_tensor(out=dst[:, :], in0=junk[:, :], scalar=RSTD, in1=gbc[:, :], op0=ALU.mult,
                                             op1=ALU.mult), reads=[junk_key, st_key, gk], writes=[dst_key])
    sch.dve(lambda v: v.tensor_tensor(out=dst[:, :], in0=dst[:, :], in1=bbc[:, :], op=ALU.add),
            reads=[dst_key, bk], writes=[dst_key])


def _consts():
    ident = np.eye(128, dtype=np.float32)
    p64 = np.zeros((128, 128), np.float32)
    for m in range(128):
        if (m % 64) < 32:
            p64[m + 32, m] = -1.0
        else:
            p64[m - 32, m] = 1.0
    p16 = np.zeros((128, 128), np.float32)
    for m in range(64, 96):
        if (m - 64) < 16:
            p16[m + 16, m] = -1.0
        else:
            p16[m - 16, m] = 1.0
    tri = np.where(np.arange(128)[None, :] <= np.arange(128)[:, None], 0.0, NEG).astype(np.float32)
    triT = np.zeros((128, 4, 512), np.float32)
    for j in range(4):
        s = j * 128 + np.arange(128)[:, None]
        t = np.arange(512)[None, :]
        triT[:, j, :] = np.where(s <= t, 0.0, NEG)
    p = np.arange(128)
    invf = np.stack([
        (np.float32(10000.0) ** (-(p % 32).astype(np.float32) / np.float32(32))).astype(np.float32),
        (np.float32(10000.0) ** (-(p % 16).astype(np.float32) / np.float32(16))).astype(np.float32)], axis=1)
    krow = np.zeros((128, 16), np.float32)
    return dict(c_identb=ident, c_perm64=p64, c_perm16=p16, c_tri=tri, c_triT=triT.reshape(128, 2048),
                c_invf=invf.astype(np.float32), c_krow=krow)


_CACHE = {}


def kernel(x, positions, w_in, b_gate, rms_cq, rms_ckv, w_uq, w_ukv, w_o_a, w_o_b, w_out, ln1_g, ln1_b,
           w_router, b_router, w_up, b_up, w_down, b_down, ln2_g, ln2_b, _dbg=None, _stop=4):
    f = lambda a: np.ascontiguousarray(np.asarray(a), dtype=np.float32)
    x = f(x)
    positions = np.ascontiguousarray(np.asarray(positions), dtype=np.int32)
    key = (tuple(sorted((_dbg or {}).items())), _stop)
    if key not in _CACHE:
        _CACHE[key] = build_program(_dbg, _stop)
    nc = _CACHE[key]
    w_ukv_ = f(w_ukv)[0].reshape(256, 8, 128)
    shared = dict(
        w_in=f(w_in)[0],
        b_gate=np.ascontiguousarray(f(b_gate)[0].reshape(16, 128).T),
        rms_cq=np.ascontiguousarray(f(rms_cq)[0].reshape(3, 128).T),
        rms_ckv=np.ascontiguousarray(f(rms_ckv)[0].reshape(2, 128).T),
        w_uq=f(w_uq)[0],
        w_ukv_k=np.ascontiguousarray(w_ukv_[:, :, 0:64].reshape(256, 512)),
        w_ukv_v=np.ascontiguousarray(w_ukv_[:, :, 64:128].reshape(256, 512)),
        w_o_a=f(w_o_a)[0], w_o_b=f(w_o_b)[0], w_out=f(w_out)[0],
        ln1_g=f(ln1_g)[0].reshape(1, D), ln1_b=f(ln1_b)[0].reshape(1, D),
        w_router=f(w_router)[0], b_router=f(b_router)[0].reshape(1, E),
        w_up=f(w_up)[0],
        b_up=np.ascontiguousarray(f(b_up)[0].reshape(E, 16, 128).transpose(2, 0, 1).reshape(128, E * 16)),
        w_down=f(w_down)[0], b_down=f(b_down)[0],
        ln2_g=f(ln2_g)[0].reshape(1, D), ln2_b=f(ln2_b)[0].reshape(1, D),
    )
    shared.update(_consts())
    in_maps = []
    for c in range(NCORES):
        m = dict(shared)
        m["x"] = x[c]
        m["pos"] = positions[c].reshape(1, S)
        in_maps.append(m)
    res = run_bass_kernel_spmd(nc, in_maps, core_ids=list(range(NCORES)))
    out = np.stack([res.results[c]["y"] for c in range(NCORES)], axis=0).astype(np.float32)
    if _dbg:
        kernel.last_dbg = [{k: res.results[c]["dbg_" + k] for k in _dbg} for c in range(NCORES)]
    return out
```

```python
import numpy as np
from contextlib import ExitStack
import concourse.bass as bass
import concourse.mybir as mybir
from concourse.bass_utils import run_bass_kernel_spmd

F32 = mybir.dt.float32
BF16 = mybir.dt.bfloat16
I32 = mybir.dt.int32
ALU = mybir.AluOpType
AF = mybir.ActivationFunctionType
AXX = mybir.AxisListType.X

S = 2048
D = 1024
NCORES = 8
E = 32
DFF = 1024
OFF_QA, OFF_KA, OFF_VA, OFF_QI, OFF_KI, OFF_WI, OFF_CQ, OFF_CKV, OFF_KR, OFF_G = (
    0, 512, 1024, 1536, 2048, 2112, 2120, 2504, 2760, 2792)
D_IN = 4840
NEG = -30000.0
TWO_PI = float(2 * np.pi)
DN_ALPHA = float(2.0 ** 0.25)
N_BISECT = 20
import os
KSUB = int(os.environ.get('KSUB', '9'))
KLAT = int(os.environ.get('KLAT', '9'))
KSKIP1 = int(os.environ.get('KSKIP1', '0'))
COMPUTE = ("pe", "act", "dve", "pool")


class Rec:
    def __init__(self):
        self.call = None

    def __getattr__(self, name):
        def f(*a, **k):
            self.call = (name, a, k)
            return self
        return f


class Op:
    __slots__ = ("eng", "fn", "deps", "sig", "sigval", "dma_key", "dma_cnt")

    def __init__(self, eng, fn):
        self.eng = eng
        rec = Rec()
        fn(rec)
        self.fn = rec.call
        self.deps = {}
        self.sig = False
        self.sigval = 0
        self.dma_key = None
        self.dma_cnt = 0


class Sched:
    def __init__(self, nc, eng_sems, dma_sem_pool):
        self.nc = nc
        self.eng_sems = eng_sems
        self.pool = list(dma_sem_pool)
        self.cnt = {e: 0 for e in COMPUTE}
        self.dma_sem = {}
        self.dma_cnt = {}
        self.seen = {e: {} for e in ("pe", "act", "dve", "pool", "sp")}
        self.reset()

    def reset(self):
        self.ops = {e: [] for e in ("pe", "act", "dve", "pool", "sp")}
        self.lastw = {}
        self.reads = {}

    def _add(self, eng, fn, reads, writes, dma_key=None):
        op = Op(eng, fn)
        idx = len(self.ops[eng])
        if dma_key is not None:
            if dma_key not in self.dma_sem:
                self.dma_sem[dma_key] = self.pool.pop()
                self.dma_cnt[dma_key] = 0
            self.dma_cnt[dma_key] += 16
            op.dma_key = dma_key
            op.dma_cnt = self.dma_cnt[dma_key]
            mysrc, mytok = ("dma", dma_key), op.dma_cnt
        else:
            mysrc, mytok = eng, idx
        deps = {}

        def merge(d):
            for s, t in d.items():
                if s not in deps or deps[s] < t:
                    deps[s] = t

        for r in reads:
            merge(self.lastw.get(r, {}))
            if (r if isinstance(r, str) else r[0]).startswith("ps"):
                merge({s_: t_ for s_, t_ in self.reads.get(r, {}).items() if s_ != mysrc})
        for w in writes:
            merge(self.lastw.get(w, {}))
            merge(self.reads.get(w, {}))
        if dma_key is None and eng in deps:
            if eng == "pe" or deps[eng] < idx - 1:
                del deps[eng]
        if mysrc in deps and dma_key is not None:
            del deps[mysrc]
        op.deps = deps
        for r in reads:
            self.reads.setdefault(r, {})[mysrc] = mytok
        for w in writes:
            self.lastw[w] = {mysrc: mytok}
            self.reads[w] = {}
        self.ops[eng].append(op)
        return op

    def pe(self, fn, reads=(), writes=()):
        return self._add("pe", fn, reads, writes)

    def act(self, fn, reads=(), writes=()):
        return self._add("act", fn, reads, writes)

    def dve(self, fn, reads=(), writes=()):
        return self._add("dve", fn, reads, writes)

    def gp(self, fn, reads=(), writes=()):
        return self._add("pool", fn, reads, writes)

    def dma(self, eng, out, in_, key, reads=(), writes=()):
        return self._add(eng, lambda e: e.dma_start(out=out, in_=in_), reads, writes, dma_key=key)

    def flush(self):
        ops = self.ops
        for e in ops:
            for op in ops[e]:
                for s, t in op.deps.items():
                    if isinstance(s, str):
                        ops[s][t].sig = True
        final = {}
        for e in COMPUTE:
            last = None
            for op in ops[e]:
                if op.dma_key is None:
                    last = op
            if last is not None:
                last.sig = True
            run = self.cnt[e]
            for op in ops[e]:
                if op.dma_key is None and op.sig:
                    run += 1
                    op.sigval = run
            self.cnt[e] = run
            final[e] = run
        dma_final = dict(self.dma_cnt)

        def emit(ename, eobj):
            seen = self.seen[ename]
            for op in ops[ename]:
                for s, t in op.deps.items():
                    if isinstance(s, str):
                        val = ops[s][t].sigval
                        sem = self.eng_sems[s]
                    else:
                        val = dma_final[s[1]] if s[1] in ("c0", "c1") else t
                        sem = self.dma_sem[s[1]]
                    if seen.get(s, 0) < val:
                        eobj.wait_ge(sem, val)
                        seen[s] = val
                name_, a_, k_ = op.fn
                inst = getattr(eobj, name_)(*a_, **k_)
                if op.dma_key is not None:
                    inst.then_inc(self.dma_sem[op.dma_key], 16)
                elif op.sig:
                    inst.then_inc(self.eng_sems[ename], 1)
            for s in COMPUTE:
                if s != ename and seen.get(s, 0) < final[s]:
                    eobj.wait_ge(self.eng_sems[s], final[s])
                    seen[s] = final[s]
            for k, v in dma_final.items():
                s = ("dma", k)
                if seen.get(s, 0) < v:
                    eobj.wait_ge(self.dma_sem[k], v)
                    seen[s] = v

        with self.nc.Block() as blk:
            @blk.tensor
            def _(t):
                emit("pe", t)

            @blk.scalar
            def _(a):
                emit("act", a)

            @blk.vector
            def _(v):
                emit("dve", v)

            @blk.gpsimd
            def _(g):
                emit("pool", g)

            @blk.sync
            def _(sp):
                emit("sp", sp)
        self.reset()


class Stream:
    def __init__(self, sch, name, slots, loads, eng="pool"):
        self.sch, self.name, self.slots, self.loads, self.eng = sch, name, slots, loads, eng
        self.issued = 0
        self.consumed = 0

    def _issue(self, i):
        sl = i % len(self.slots)
        for (o, a) in self.loads[i](self.slots[sl]):
            self.sch.dma(self.eng, o, a, key=(self.name, sl), writes=[(self.name, sl)])

    def next(self):
        n = len(self.slots)
        while self.issued < min(len(self.loads), self.consumed + n):
            self._issue(self.issued)
            self.issued += 1
        sl = self.consumed % n
        self.consumed += 1
        return self.slots[sl], (self.name, sl)


class _Stop(Exception):
    pass


def build_program(dbg=None, stop_after=4):
    dbg = dbg or {}
    nc = bass.Bass("TRN2", target_bir_lowering=False)
    dt_ = {}

    def din(name, shape, dtype=F32):
        h = nc.dram_tensor(name, list(shape), dtype, kind="ExternalInput")
        dt_[name] = h
        return h

    x_h = din("x", [S, D])
    pos_h = din("pos", [1, S], I32)
    w_in_h = din("w_in", [D, D_IN])
    bgate_h = din("b_gate", [128, 16])
    rmscq_h = din("rms_cq", [128, 3])
    rmsckv_h = din("rms_ckv", [128, 2])
    wuq_h = din("w_uq", [384, 768])
    wukv_k_h = din("w_ukv_k", [256, 512])
    wukv_v_h = din("w_ukv_v", [256, 512])
    woa_h = din("w_o_a", [512, D])
    wob_h = din("w_o_b", [512, D])
    wout_h = din("w_out", [D, D])
    ln1g_h = din("ln1_g", [1, D])
    ln1b_h = din("ln1_b", [1, D])
    wr_h = din("w_router", [D, E])
    br_h = din("b_router", [1, E])
    wup_h = din("w_up", [E, D, 2 * DFF])
    bup_h = din("b_up", [128, E * 16])
    wdn_h = din("w_down", [E, DFF, D])
    bdn_h = din("b_down", [E, D])
    ln2g_h = din("ln2_g", [1, D])
    ln2b_h = din("ln2_b", [1, D])
    c_identb = din("c_identb", [128, 128])
    c_perm64 = din("c_perm64", [128, 128])
    c_perm16 = din("c_perm16", [128, 128])
    c_tri = din("c_tri", [128, 128])
    c_triT = din("c_triT", [128, 4 * 512])
    c_invf = din("c_invf", [128, 2])
    c_krow = din("c_krow", [128, 16])

    y_h = nc.dram_tensor("y", [S, D], F32, kind="ExternalOutput")
    hs_h = y_h
    dbg_out = {}
    for k, shp in dbg.items():
        dbg_out[k] = nc.dram_tensor("dbg_" + k, list(shp), F32, kind="ExternalOutput")

    es = ExitStack()
    try:
      with es:
          def sb(name, shape, dtype, stack=es):
              return stack.enter_context(nc.sbuf_tensor(name, list(shape), dtype))

          def psum(name, shape, dtype, stack):
              return stack.enter_context(nc.psum_tensor(name, list(shape), dtype))

          eng_sems = {e: es.enter_context(nc.semaphore("sem_" + e)) for e in COMPUTE}
          dma_pool = [es.enter_context(nc.semaphore("dsem%d" % i)) for i in range(40)]
          sch = Sched(nc, eng_sems, dma_pool)

          identb = sb("identb", [128, 128], BF16)
          identf = sb("identf", [128, 128], F32)
          perm64 = sb("perm64", [128, 128], F32)
          perm16 = sb("perm16", [128, 128], F32)
          onesb = sb("onesb", [128, 128], BF16)
          onesf = sb("onesf", [128, 128], F32)
          tri = sb("tri", [128, 128], F32)
          triT = sb("triT", [128, 4, 512], BF16)
          invf = sb("invf", [128, 2], F32)
          krow = sb("krow", [128, 16], F32)
          epsln = sb("epsln", [128, 1], F32)
          epsrms = sb("epsrms", [128, 1], F32)
          negpi = sb("negpi", [128, 1], F32)
          XH = sb("XH", [128, 8, S], BF16)
          comb = sb("comb", [128, 16, E], F32)

          sch.dma("pool", identb[:], c_identb.ap(), key="c0", writes=["identb"])
          sch.dma("sp", identf[:], c_identb.ap(), key="c1", writes=["identf"])
          sch.dma("sp", perm64[:], c_perm64.ap(), key="c1", writes=["perm64"])
          sch.dma("sp", perm16[:], c_perm16.ap(), key="c1", writes=["perm16"])
          sch.dma("sp", tri[:], c_tri.ap(), key="c1", writes=["tri"])
          sch.dma("pool", triT[:].rearrange("p a b -> p (a b)"), c_triT.ap(), key="c0", writes=["triT"])
          sch.dma("sp", invf[:], c_invf.ap(), key="c1", writes=["invf"])
          sch.dma("sp", krow[:], c_krow.ap(), key="c1", writes=["krow"])
          sch.dve(lambda v: v.memset(onesb[:], 1.0), writes=["onesb"])
          sch.dve(lambda v: v.memset(onesf[:], 1.0), writes=["onesf"])
          sch.dve(lambda v: v.memset(epsln[:], 1e-5), writes=["epsln"])
          sch.dve(lambda v: v.memset(epsrms[:], 1e-6), writes=["epsrms"])
          sch.dve(lambda v: v.memset(negpi[:], -float(np.pi)), writes=["negpi"])

          w_in_v = w_in_h.ap().rearrange("(kc p) n -> p kc n", p=128)

          def dump(name, src_ap, reads, rows=None):
              if name in dbg_out:
                  d = dbg_out[name].ap()
                  sch.dma("sp", d if rows is None else d[rows[0]:rows[1]], src_ap, key="dbg", reads=reads)

          def build_tables(tc, col, posi, posf, ang, u, ki, Ct, St, ukey="u", kikey="ki"):
              pos_bc = bass.AP(pos_h, tc * 512, [[0, 128], [1, 512]])
              sch.dma("sp", posi[:], pos_bc, key="pos", writes=["posi"])
              sch.dve(lambda v: v.tensor_copy(out=posf[:], in_=posi[:]), reads=["posi"], writes=["posf"])
              sch.dve(lambda v: v.tensor_scalar(out=ang[:], in0=posf[:], scalar1=invf[:, col:col + 1], scalar2=None,
                                                op0=ALU.mult), reads=["posf", "invf"], writes=["ang"])
              for (shift, T, nm) in ((0.0, St, "S"), (float(np.pi / 2), Ct, "C")):
                  sch.dve(lambda v, shift=shift: v.tensor_scalar(out=posf[:], in0=ang[:], scalar1=shift, scalar2=None,
                                                                 op0=ALU.add), reads=["ang", "posf"], writes=["posf"])
                  sch.dve(lambda v: v.tensor_scalar(out=u[:], in0=posf[:], scalar1=1.0 / TWO_PI, scalar2=None,
                                                    op0=ALU.mult), reads=["posf"], writes=[ukey])
                  sch.dve(lambda v: v.tensor_copy(out=ki[:], in_=u[:]), reads=[ukey], writes=[kikey])
                  sch.dve(lambda v: v.tensor_copy(out=u[:], in_=ki[:]), reads=[kikey], writes=[ukey])
                  sch.dve(lambda v: v.scalar_tensor_tensor(out=u[:], in0=u[:], scalar=-TWO_PI, in1=posf[:],
                                                           op0=ALU.mult, op1=ALU.add), reads=[ukey, "posf"], writes=[ukey])
                  sch.dve(lambda v: v.tensor_scalar(out=u[:], in0=u[:], scalar1=float(np.pi) - 1e-6,
                                                    scalar2=-float(np.pi) + 1e-6, op0=ALU.min, op1=ALU.max),
                          reads=[ukey], writes=[ukey])
                  sch.act(lambda a, T=T: a.activation(out=T[:], in_=u[:], func=AF.Sin), reads=[ukey], writes=[("tab", nm)])

          def rope_block(ps, ps_key, r0, r1, perm, permkey, psR, psR_key, zs, t1, Ct, St, dsts, dst_keys):
              sch.act(lambda a: a.copy(out=zs[r0:r1, :], in_=ps[r0:r1, :]), reads=[ps_key], writes=["zs"])
              sch.pe(lambda t: t.matmul(psR[:, :], lhsT=perm[:, :], rhs=zs[:, :], start=True, stop=True),
                     reads=["zs", permkey], writes=[psR_key])
              sch.dve(lambda v: v.tensor_tensor(out=t1[r0:r1, :], in0=zs[r0:r1, :], in1=Ct[r0:r1, :], op=ALU.mult),
                      reads=["zs", ("tab", "C")], writes=["t1"])
              sch.dve(lambda v: v.tensor_tensor(out=zs[r0:r1, :], in0=psR[r0:r1, :], in1=St[r0:r1, :], op=ALU.mult),
                      reads=[psR_key, ("tab", "S")], writes=["zs"])
              for d, dk in zip(dsts, dst_keys):
                  sch.dve(lambda v, d=d: v.tensor_tensor(out=d, in0=t1[r0:r1, :], in1=zs[r0:r1, :], op=ALU.add),
                          reads=["t1", "zs"], writes=[dk])

          def attn_chunk(nst, heads, psL, psO2, psD2, pT, rden, scale):
              steps = [(hi, st) for hi in range(len(heads)) for st in range(nst)]

              def emit_L(i):
                  hi, st = steps[i]
                  H = heads[hi]
                  L = psL[i % 2]
                  Lk = ("psG", i % 2)
                  b = H["bias_fn"](st)
                  sch.pe(lambda t: t.matmul(L[:, :], lhsT=H["kT_fn"](st), rhs=H["qT_fn"](), start=True, stop=(b is None)),
                         reads=H["kq_reads"], writes=[Lk])
                  if b is not None:
                      sch.pe(lambda t: t.matmul(L[:, :], lhsT=identb[:, :], rhs=b[0], start=False, stop=True),
                             reads=["identb"] + b[1], writes=[Lk])
                  P = pT[i % 3]
                  sch.act(lambda a: a.activation(out=P[:, :], in_=L[:, :], func=AF.Exp, scale=scale),
                          reads=[Lk], writes=[("pT", i % 3)])

              def emit_PV(i):
                  hi, st = steps[i]
                  H = heads[hi]
                  P = pT[i % 3]
                  Pk = ("pT", i % 3)
                  O = psO2[hi % 2]
                  Dn = psD2[hi % 2]
                  sch.pe(lambda t: t.matmul(O[:, :], lhsT=H["v_fn"](st), rhs=P[:, :], start=(st == 0), stop=(st == nst - 1)),
                         reads=[Pk] + H["v_reads"], writes=[("psO", hi % 2)])
                  sch.pe(lambda t: t.matmul(Dn[:, :], lhsT=onesb[:, :], rhs=P[:, :], start=(st == 0), stop=(st == nst - 1)),
                         reads=[Pk, "onesb"], writes=[("psD", hi % 2)])
                  if st == nst - 1:
                      r0, r1 = H["rows"]
                      sch.dve(lambda v: v.reciprocal(out=rden[r0:r1, :], in_=Dn[r0:r1, :]), reads=[("psD", hi % 2)],
                              writes=["rden"])
                      sch.dve(lambda v: v.tensor_tensor(out=H["out_ap"], in0=O[r0:r1, :], in1=rden[r0:r1, :], op=ALU.mult),
                              reads=[("psO", hi % 2), "rden"], writes=[H["out_key"]])

              for i in range(len(steps) + 1):
                  if i < len(steps):
                      emit_L(i)
                  if i >= 1:
                      emit_PV(i - 1)

          es_o = ExitStack()
          with es_o:
              oaT = sb("oaT", [128, 4, S], BF16, es_o)
              obT = sb("obT", [128, 4, S], BF16, es_o)

              p1 = ExitStack()
              with p1:
                  KA = sb("KA", [128, 4, S], BF16, p1)
                  KI2 = sb("KI2", [128, S], BF16, p1)
                  VA = sb("VA", [128, 16, 512], BF16, p1)
                  QAc = sb("QAc", [128, 4, 512], BF16, p1)
                  QIc = sb("QIc", [128, 4, 512], BF16, p1)
                  score4 = [sb("score%d" % i, [128, S], F32, p1) for i in range(4)]
                  score = score4[0]
                  mb = sb("mb", [128, S], BF16, p1)
                  junk = mb
                  maskbT = sb("maskbT", [128, 16, 512], BF16, p1)
                  pT = [sb("pT%d" % i, [128, 512], BF16, p1) for i in range(3)]
                  zs = sb("zs", [128, 512], F32, p1)
                  t1 = sb("t1", [128, 512], F32, p1)
                  Ct = sb("Ct", [128, 512], F32, p1)
                  St = sb("St", [128, 512], F32, p1)
                  posi = sb("posi", [128, 512], I32, p1)
                  posf = sb("posf", [128, 512], F32, p1)
                  ang = sb("ang", [128, 512], F32, p1)
                  uu = zs
                  kk = posi
                  xb = [sb("xb%d" % i, [128, D], BF16, p1) for i in range(1)]
                  rden = sb("rden", [128, 512], F32, p1)
                  relu_t = [sb("relu%d" % i, [128, 512], F32, p1) for i in range(2)]
                  wi_t = sb("wi_t", [128, 8], F32, p1)
                  wabs = sb("wabs", [128, 8], F32, p1)
                  wsgn = sb("wsgn", [128, 8], F32, p1)
                  bs = sb("bs", [128, 24], F32, p1)
                  LO4, W4, MID4, CNT4, GE4 = (bs[:, 4 * i:4 * i + 4] for i in range(5))
                  wring = [sb("wring%d" % i, [128, 8, 256], BF16, p1) for i in range(3)]
                  psG = [psum("psG%d" % i, [128, 512], F32, p1) for i in range(2)]
                  psO = [psum("psO%d" % i, [128, 512], F32, p1) for i in range(2)]
                  psD = [psum("psD%d" % i, [128, 512], F32, p1) for i in range(2)]
                  psT = psum("psT", [128, 1024], BF16, p1)
                  psR = psum("psR", [128, 512], F32, p1)

                  def ld_cols(c0, n, dst0=0):
                      def f(slot):
                          return [(slot[:, :, dst0:dst0 + n], w_in_v[:, :, c0:c0 + n])]
                      return f

                  def ld_ki(slot):
                      return [(slot[:, :, 0:64], w_in_v[:, :, OFF_KI:OFF_KI + 64]),
                              (slot[:, :, 64:128], w_in_v[:, :, OFF_KI:OFF_KI + 64])]

                  loads1 = []
                  for tc in range(4):
                      for off in (OFF_QA, OFF_KA, OFF_QI):
                          loads1 += [ld_cols(off, 256), ld_cols(off + 256, 256)]
                      loads1 += [ld_ki]
                      loads1 += [ld_cols(OFF_VA, 256), ld_cols(OFF_VA + 256, 256)]
                      loads1 += [ld_cols(OFF_WI, 8)]
                  ws1 = Stream(sch, "wring", wring, loads1)

                  gcount = [0]
                  scount = [0]

                  def nextG():
                      i = gcount[0] % 2
                      gcount[0] += 1
                      return psG[i], ("psG", i)

                  for tc in range(0 if KSKIP1 else 4):
                      tok0 = tc * 512
                      for j in range(4):
                          tt = tc * 4 + j
                          xbj = xb[0]
                          sch.dma("pool", xbj[:], x_h.ap()[tt * 128:(tt + 1) * 128, :], key=("xb", 0),
                                  writes=[("xb", 0)])
                          for half in range(2):
                              for k in range(4):
                                  kc = half * 4 + k
                                  sch.pe(lambda t, kc=kc, k=k, xbj=xbj: t.transpose(
                                      out=psT[:, k * 128:(k + 1) * 128], in_=xbj[:, kc * 128:(kc + 1) * 128],
                                      identity=identb[:, :]), reads=[("xb", 0), "identb"], writes=["psT"])
                              sch.act(lambda a, half=half, tt=tt: a.copy(
                                  out=XH[:, half * 4:half * 4 + 4, tt * 128:(tt + 1) * 128],
                                  in_=psT[:, 0:512].rearrange("p (k t) -> p k t", k=4)),
                                  reads=["psT"], writes=[("XH", tc)])
                      build_tables(tc, 0, posi, posf, ang, uu, kk, Ct, St, ukey="zs", kikey="posi")

                      def proj_fm(ncols_chunks, dst_fn, dst_key):
                          for g in range(ncols_chunks // 2):
                              slot, sk = ws1.next()
                              for c2 in range(2):
                                  cc = g * 2 + c2
                                  G, Gk = nextG()
                                  for kc in range(8):
                                      sch.pe(lambda t, G=G, kc=kc, c2=c2, slot=slot: t.matmul(
                                          G[:, :], lhsT=slot[:, kc, c2 * 128:(c2 + 1) * 128],
                                          rhs=XH[:, kc, tok0:tok0 + 512], start=(kc == 0), stop=(kc == 7)),
                                          reads=[sk, ("XH", tc)], writes=[Gk])
                                  rope_block(G, Gk, 0, 128, perm64, "perm64", psR, "psR", zs, t1, Ct, St,
                                             [dst_fn(cc)], [dst_key(cc)])

                      proj_fm(4, lambda cc: QAc[:, cc, :], lambda cc: ("QAc", cc))
                      proj_fm(4, lambda cc: KA[:, cc, tok0:tok0 + 512], lambda cc: ("KA", cc, tc))
                      proj_fm(4, lambda cc: QIc[:, cc, :], lambda cc: ("QIc", cc))
                      slot, sk = ws1.next()
                      G, Gk = nextG()
                      for kc in range(8):
                          sch.pe(lambda t, G=G, kc=kc, slot=slot: t.matmul(
                              G[:, :], lhsT=slot[:, kc, 0:128], rhs=XH[:, kc, tok0:tok0 + 512],
                              start=(kc == 0), stop=(kc == 7)), reads=[sk, ("XH", tc)], writes=[Gk])
                      rope_block(G, Gk, 0, 128, perm64, "perm64", psR, "psR", zs, t1, Ct, St,
                                 [KI2[:, tok0:tok0 + 512]], [("KI2", tc)])
                      for g in range(2):
                          slot, sk = ws1.next()
                          for j in range(4):
                              tt = tc * 4 + j
                              G, Gk = nextG()
                              for kc in range(8):
                                  sch.pe(lambda t, G=G, kc=kc, slot=slot, tt=tt: t.matmul(
                                      G[:, 0:256], lhsT=XH[:, kc, tt * 128:(tt + 1) * 128], rhs=slot[:, kc, 0:256],
                                      start=(kc == 0), stop=(kc == 7)), reads=[sk, ("XH", tc)], writes=[Gk])
                              sch.act(lambda a, G=G, tt=tt, g=g: a.copy(out=VA[:, tt, g * 256:(g + 1) * 256],
                                                                        in_=G[:, 0:256]),
                                      reads=[Gk], writes=[("VA", tt, g)])
                      slot_wi, sk_wi = ws1.next()

                      n_c = (tc + 1) * 512
                      cs = float((64 ** -0.5) * (8 ** -0.5))
                      for j in range(4):
                          tt = tc * 4 + j
                          n_s = (tt + 1) * 128
                          SC = score4[j]
                          SCk = ("score", j)
                          G, Gk = nextG()
                          for kc in range(8):
                              sch.pe(lambda t, G=G, kc=kc, tt=tt: t.matmul(
                                  G[:, 0:8], lhsT=XH[:, kc, tt * 128:(tt + 1) * 128], rhs=slot_wi[:, kc, 0:8],
                                  start=(kc == 0), stop=(kc == 7)), reads=[sk_wi, ("XH", tc)], writes=[Gk])
                          sch.dve(lambda v, G=G: v.tensor_copy(out=wi_t[:, :], in_=G[:, 0:8]), reads=[Gk], writes=["wi_t"])
                          sch.dve(lambda v: v.tensor_scalar(out=wsgn[:, :], in0=wi_t[:, :], scalar1=0.0, scalar2=2.0,
                                                            op0=ALU.is_ge, op1=ALU.mult), reads=["wi_t"], writes=["wsgn"])
                          sch.dve(lambda v: v.tensor_scalar(out=wsgn[:, :], in0=wsgn[:, :], scalar1=-1.0, scalar2=None,
                                                            op0=ALU.add), reads=["wsgn"], writes=["wsgn"])
                          sch.dve(lambda v: v.tensor_tensor(out=wabs[:, :], in0=wi_t[:, :], in1=wsgn[:, :], op=ALU.mult),
                                  reads=["wi_t", "wsgn"], writes=["wabs"])
                          sch.dve(lambda v: v.tensor_scalar(out=wabs[:, :], in0=wabs[:, :], scalar1=cs, scalar2=None,
                                                            op0=ALU.mult), reads=["wabs"], writes=["wabs"])
                          nblk = (n_s + 511) // 512
                          rcount = 0
                          sbanks = [(psG[0], ("psG", 0)), (psG[1], ("psG", 1)), (psO[0], ("psO", 0)), (psO[1], ("psO", 1)),
                                    (psD[0], ("psD", 0)), (psD[1], ("psD", 1))]
                          rbufs = [(relu_t[0], ("relu", 0)), (relu_t[1], ("relu", 1)), (rden, "rden"), (t1, "t1")]
                          for sbk in range(nblk):
                              s0 = sbk * 512
                              w = min(512, n_s - s0)
                              for h in range(8):
                                  hp = (h % 2) * 64
                                  G, Gk = sbanks[scount[0] % len(sbanks)]
                                  scount[0] += 1
                                  sch.pe(lambda t, G=G, h=h, hp=hp, s0=s0, w=w, j=j: t.matmul(
                                      G[:, 0:w], lhsT=QIc[hp:hp + 64, h // 2, j * 128:(j + 1) * 128],
                                      rhs=KI2[hp:hp + 64, s0:s0 + w], start=True, stop=True),
                                      reads=[("QIc", h // 2)] + [("KI2", c) for c in range(tc + 1)], writes=[Gk])
                                  R, Rk = rbufs[scount[0] % len(rbufs)]
                                  sch.act(lambda a, G=G, R=R, h=h, w=w: a.activation(
                                      out=R[:, 0:w], in_=G[:, 0:w], func=AF.Relu, scale=wabs[:, h:h + 1]),
                                      reads=[Gk, "wabs"], writes=[Rk])
                                  if h == 0:
                                      sch.dve(lambda v, R=R, s0=s0, w=w, SC=SC: v.tensor_scalar(
                                          out=SC[:, s0:s0 + w], in0=R[:, 0:w], scalar1=wsgn[:, 0:1], scalar2=None,
                                          op0=ALU.mult), reads=[Rk, "wsgn"], writes=[SCk])
                                  else:
                                      sch.dve(lambda v, R=R, s0=s0, w=w, h=h, SC=SC: v.scalar_tensor_tensor(
                                          out=SC[:, s0:s0 + w], in0=R[:, 0:w], scalar=wsgn[:, h:h + 1],
                                          in1=SC[:, s0:s0 + w], op0=ALU.mult, op1=ALU.add),
                                          reads=[Rk, "wsgn", SCk], writes=[SCk])
                          sch.dve(lambda v, tt=tt, SC=SC: v.tensor_tensor(
                              out=SC[:, tt * 128:(tt + 1) * 128], in0=SC[:, tt * 128:(tt + 1) * 128], in1=tri[:, :],
                              op=ALU.add), reads=[SCk, "tri"], writes=[SCk])
                          if tt == 5:
                              dump("score5", SC[:, 0:n_s], [SCk])
                          if tt >= 2:
                              sch.dve(lambda v, tt=tt, SC=SC, j=j: v.tensor_reduce(out=LO4[:, j:j + 1], in_=SC[:, 0:tt * 128],
                                                                                  axis=AXX, op=ALU.min),
                                      reads=[SCk], writes=[("lo", j)])
                              sch.dve(lambda v, n_s=n_s, SC=SC, j=j: v.tensor_reduce(out=W4[:, j:j + 1], in_=SC[:, 0:n_s],
                                                                                    axis=AXX, op=ALU.max),
                                      reads=[SCk], writes=[("w", j)])
                              sch.dve(lambda v, j=j: v.scalar_tensor_tensor(
                                  out=W4[:, j:j + 1], in0=W4[:, j:j + 1], scalar=1.0, in1=LO4[:, j:j + 1], op0=ALU.add,
                                  op1=ALU.subtract), reads=[("w", j), ("lo", j)], writes=[("w", j)])
                      j0 = 2 if tc == 0 else 0
                      cols = slice(j0, 4)
                      lo_keys = [("lo", j) for j in range(j0, 4)]
                      w_keys = [("w", j) for j in range(j0, 4)]
                      for it in range(N_BISECT):
                          sch.dve(lambda v: v.tensor_scalar(out=W4[:, cols], in0=W4[:, cols], scalar1=0.5, scalar2=None,
                                                            op0=ALU.mult), reads=w_keys, writes=w_keys)
                          sch.dve(lambda v: v.tensor_tensor(out=MID4[:, cols], in0=LO4[:, cols], in1=W4[:, cols], op=ALU.add),
                                  reads=lo_keys + w_keys, writes=["mid"])
                          sch.dve(lambda v: v.memset(CNT4[:, cols], 0.0), reads=["ge"], writes=[("cnt", j) for j in range(j0, 4)])
                          for j in range(j0, 4):
                              n_s = (tc * 4 + j + 1) * 128
                              sch.dve(lambda v, n_s=n_s, j=j: v.tensor_scalar(
                                  out=junk[:, 0:n_s], in0=score4[j][:, 0:n_s], scalar1=MID4[:, j:j + 1], scalar2=0.0,
                                  op0=ALU.is_ge, op1=ALU.add, accum_out=CNT4[:, j:j + 1]),
                                  reads=[("score", j), "mid", ("cnt", j)], writes=[("cnt", j)])
                          sch.dve(lambda v: v.tensor_scalar(out=GE4[:, cols], in0=CNT4[:, cols], scalar1=255.5, scalar2=None,
                                                            op0=ALU.is_ge), reads=[("cnt", j) for j in range(j0, 4)],
                                  writes=["ge"])
                          sch.dve(lambda v: v.tensor_tensor(out=GE4[:, cols], in0=GE4[:, cols], in1=W4[:, cols], op=ALU.mult),
                                  reads=["ge"] + w_keys, writes=["ge"])
                          sch.dve(lambda v: v.tensor_tensor(out=LO4[:, cols], in0=LO4[:, cols], in1=GE4[:, cols], op=ALU.add),
                                  reads=["ge"] + lo_keys, writes=lo_keys)
                      if tc == 0:
                          sch.dve(lambda v: v.memset(LO4[:, 0:2], -20000.0), writes=[("lo", 0), ("lo", 1)])
                      for j in range(4):
                          tt = tc * 4 + j
                          n_s = (tt + 1) * 128
                          sch.dve(lambda v, n_s=n_s, j=j: v.tensor_scalar(
                              out=mb[:, 0:n_s], in0=score4[j][:, 0:n_s], scalar1=LO4[:, j:j + 1], scalar2=NEG, op0=ALU.is_lt,
                              op1=ALU.mult), reads=[("score", j), ("lo", j)], writes=["mb"])
                          if n_c > n_s:
                              sch.dve(lambda v, n_s=n_s: v.memset(mb[:, n_s:n_c], NEG), reads=["mb"], writes=["mb"])
                          for sg in range(tc + 1):
                              for k in range(4):
                                  sch.pe(lambda t, sg=sg, k=k: t.transpose(
                                      out=psT[:, k * 128:(k + 1) * 128],
                                      in_=mb[:, (sg * 4 + k) * 128:(sg * 4 + k + 1) * 128], identity=identb[:, :]),
                                      reads=["mb", "identb"], writes=["psT"])
                              sch.act(lambda a, sg=sg, j=j: a.copy(
                                  out=maskbT[:, sg * 4:sg * 4 + 4, j * 128:(j + 1) * 128],
                                  in_=psT[:, 0:512].rearrange("p (k t) -> p k t", k=4)),
                                  reads=["psT"], writes=[("maskbT", sg)])
                      nst = (tc + 1) * 4
                      heads = []
                      for h in range(8):
                          hp = (h % 2) * 64
                          cc = h // 2
                          heads.append(dict(
                              kT_fn=lambda st, hp=hp, cc=cc: KA[hp:hp + 64, cc, st * 128:(st + 1) * 128],
                              qT_fn=lambda hp=hp, cc=cc: QAc[hp:hp + 64, cc, :],
                              kq_reads=[("KA", cc, c) for c in range(tc + 1)] + [("QAc", cc)],
                              bias_fn=lambda st: (maskbT[:, st, :], [("maskbT", st // 4)]),
                              v_fn=lambda st, cc=cc: VA[:, st, cc * 128:(cc + 1) * 128],
                              v_reads=[("VA", t_, cc // 2) for t_ in range(nst)],
                              rows=(hp, hp + 64),
                              out_ap=oaT[hp:hp + 64, cc, tok0:tok0 + 512], out_key=("oaT", cc, tc, h % 2)))
                      attn_chunk(nst, heads, psG, psO, psD, pT, rden, float(64 ** -0.5))
                  if "oaT" in dbg_out:
                      for cc in range(4):
                          sch.dve(lambda v, cc=cc: v.tensor_copy(out=score[:, :], in_=oaT[:, cc, :]),
                                  reads=[("oaT", cc, c, q) for c in range(4) for q in range(2)], writes=["score"])
                          dump("oaT", score[:, :], ["score"], rows=(cc * 128, (cc + 1) * 128))
                  sch.flush()
                  if stop_after == 1:
                      raise _Stop()

              p2 = ExitStack()
              with p2:
                  Kh = sb("Kh", [128, 8, S], BF16, p2)
                  Vb = sb("Vb", [128, 16, 512], BF16, p2)
                  Qh = sb("Qh", [128, 8, 512], BF16, p2)
                  cqg = sb("cqg", [128, 3, 512], BF16, p2)
                  ckvg = sb("ckvg", [128, 2, 512], BF16, p2)
                  sq = sb("sq", [128, 512], F32, p2)
                  sqv = sb("sqv", [128, 2, 512], F32, p2)
                  rq_bc = sb("rq_bc", [128, 512], F32, p2)
                  rkv_bc = sb("rkv_bc", [128, 512], F32, p2)
                  rkv_tok = sb("rkv_tok", [128, 4], F32, p2)
                  pT = [sb("pT2_%d" % i, [128, 512], BF16, p2) for i in range(3)]
                  zs = sb("zs2", [128, 512], F32, p2)
                  t1 = sb("t1_2", [128, 512], F32, p2)
                  Ct = sb("Ct2", [128, 512], F32, p2)
                  St = sb("St2", [128, 512], F32, p2)
                  posi = sb("posi2", [128, 512], I32, p2)
                  posf = sb("posf2", [128, 512], F32, p2)
                  ang = sb("ang2", [128, 512], F32, p2)
                  uu = sb("uu2", [128, 512], F32, p2)
                  kk = sb("kk2", [128, 512], I32, p2)
                  rden = sb("rden2", [128, 512], F32, p2)
                  wuq = sb("wuq", [128, 3, 768], BF16, p2)
                  wukvk = sb("wukvk", [128, 2, 512], BF16, p2)
                  wukvv = sb("wukvv", [128, 2, 512], BF16, p2)
                  gq = sb("gq", [128, 3], F32, p2)
                  gkv = sb("gkv", [128, 2], F32, p2)
                  wring = [sb("wring2_%d" % i, [128, 8, 256], BF16, p2) for i in range(3)]
                  psG = [psum("psG2_%d" % i, [128, 512], F32, p2) for i in range(2)]
                  psO = [psum("psO2_%d" % i, [128, 512], F32, p2) for i in range(2)]
                  psD = [psum("psD2_%d" % i, [128, 512], F32, p2) for i in range(2)]
                  psS = psum("psS2", [128, 512], F32, p2)
                  psR = psum("psR2", [128, 512], F32, p2)

                  sch.dve(lambda v: v.memset(zs[:, :], 0.0), writes=["zs"])
                  sch.dma("pool", wuq[:], wuq_h.ap().rearrange("(kc p) n -> p kc n", p=128), key="c0", writes=["wuq"])
                  sch.dma("pool", wukvk[:], wukv_k_h.ap().rearrange("(kc p) n -> p kc n", p=128), key="c0", writes=["wukvk"])
                  sch.dma("pool", wukvv[:], wukv_v_h.ap().rearrange("(kc p) n -> p kc n", p=128), key="c0", writes=["wukvv"])
                  sch.dma("sp", gq[:], rmscq_h.ap(), key="c1", writes=["gq"])
                  sch.dma("sp", gkv[:], rmsckv_h.ap(), key="c1", writes=["gkv"])

                  def ld_cols(c0, n, dst0=0):
                      def f(slot):
                          return [(slot[:, :, dst0:dst0 + n], w_in_v[:, :, c0:c0 + n])]
                      return f

                  loads2 = []
                  for tc in range(4):
                      loads2 += [ld_cols(OFF_CQ, 256), ld_cols(OFF_CQ + 256, 128), ld_cols(OFF_CKV, 256),
                                 ld_cols(OFF_KR, 32, dst0=64)]
                  ws2 = Stream(sch, "wring2", wring, loads2)
                  gcount = [0]

                  def nextG():
                      i = gcount[0] % 2
                      gcount[0] += 1
                      return psG[i], ("psG", i)

                  for tc in range(4):
                      tok0 = tc * 512
                      build_tables(tc, 1, posi, posf, ang, uu, kk, Ct, St)

                      def latent(nchunks, slots_cols, dst, dstname, gain, gain_key, nfeat, rbc, rbc_key, keep_sq, extra=None):
                          cidx = 0
                          for (ncol_chunks) in slots_cols:
                              slot, sk = ws2.next()
                              for c2 in range(ncol_chunks):
                                  G, Gk = nextG()
                                  for kc in range(8):
                                      sch.pe(lambda t, G=G, kc=kc, c2=c2, slot=slot: t.matmul(
                                          G[:, :], lhsT=slot[:, kc, c2 * 128:(c2 + 1) * 128],
                                          rhs=XH[:, kc, tok0:tok0 + 512], start=(kc == 0), stop=(kc == 7)),
                                          reads=[sk, ("XH", tc)], writes=[Gk])
                                  sqt = sqv[:, cidx, :] if keep_sq else sq[:, :]
                                  sqk = ("sqv", cidx) if keep_sq else "sq"
                                  if KLAT >= 1:
                                      sch.act(lambda a, G=G, sqt=sqt: a.activation(out=sqt, in_=G[:, :], func=AF.Square),
                                              reads=[Gk], writes=[sqk])
                                  if KLAT >= 2:
                                      sch.dve(lambda v, G=G, cidx=cidx: v.tensor_scalar(
                                          out=dst[:, cidx, :], in0=G[:, :], scalar1=gain[:, cidx:cidx + 1], scalar2=None,
                                          op0=ALU.mult), reads=[Gk, gain_key, sqk], writes=[(dstname, cidx)])
                                  if KLAT >= 3:
                                      sch.pe(lambda t, sqt=sqt, cidx=cidx: t.matmul(
                                          psS[:, :], lhsT=onesf[:, :], rhs=sqt, start=(cidx == 0), stop=(cidx == nchunks - 1)),
                                          reads=[sqk, "onesf"], writes=["psS"])
                                  cidx += 1
                              if extra is not None:
                                  extra(slot, sk)
                          if KLAT < 4:
                              return
                          sch.act(lambda a: a.activation(out=rbc[:, :], in_=psS[:, :], func=AF.Sqrt, bias=epsrms[:, 0:1],
                                                         scale=1.0 / nfeat), reads=["psS", "epsrms"], writes=[rbc_key])
                          sch.dve(lambda v: v.reciprocal(out=rbc[:, :], in_=rbc[:, :]), reads=[rbc_key], writes=[rbc_key])

                      if KSUB < 2:
                          continue
                      latent(3, [2, 1], cqg, "cqg", gq, "gq", 384.0, rq_bc, "rq_bc", False)
                      if KSUB < 3:
                          continue
                      def ckv_tok(slot, sk):
                          for j in range(4):
                              tt = tc * 4 + j
                              G, Gk = nextG()
                              for kc in range(8):
                                  sch.pe(lambda t, G=G, kc=kc, tt=tt, slot=slot: t.matmul(
                                      G[:, 0:256], lhsT=XH[:, kc, tt * 128:(tt + 1) * 128], rhs=slot[:, kc, 0:256],
                                      start=(kc == 0), stop=(kc == 7)), reads=[sk, ("XH", tc)], writes=[Gk])
                              sch.dve(lambda v, G=G: v.tensor_copy(out=sq[:, 0:256], in_=G[:, 0:256]), reads=[Gk], writes=["sq"])
                              sch.dve(lambda v: v.tensor_tensor(out=sq[:, 0:256], in0=sq[:, 0:256], in1=sq[:, 0:256],
                                                                op=ALU.mult), reads=["sq"], writes=["sq"])
                              sch.dve(lambda v, j=j: v.tensor_reduce(out=rkv_tok[:, j:j + 1], in_=sq[:, 0:256], axis=AXX,
                                                                     op=ALU.add), reads=["sq"], writes=["rkv_tok"])
                          sch.act(lambda a: a.activation(out=rkv_tok[:, :], in_=rkv_tok[:, :], func=AF.Sqrt,
                                                         bias=epsrms[:, 0:1], scale=1.0 / 256.0),
                                  reads=["rkv_tok", "epsrms"], writes=["rkv_tok"])

                      latent(2, [2], ckvg, "ckvg", gkv, "gkv", 256.0, rkv_bc, "rkv_bc", False, extra=ckv_tok)
                      sch.dve(lambda v: v.reciprocal(out=rkv_tok[:, :], in_=rkv_tok[:, :]), reads=["rkv_tok"],
                              writes=["rkv_tok"])
                      if KSUB < 4:
                          continue
                      slot, sk = ws2.next()
                      G, Gk = nextG()
                      for kc in range(8):
                          sch.pe(lambda t, G=G, kc=kc, slot=slot: t.matmul(
                              G[0:96, :], lhsT=slot[:, kc, 0:96], rhs=XH[:, kc, tok0:tok0 + 512],
                              start=(kc == 0), stop=(kc == 7)), reads=[sk, ("XH", tc)], writes=[Gk])
                      rope_block(G, Gk, 64, 96, perm16, "perm16", psR, "psR", zs, t1, Ct, St,
                                 [Kh[64:96, h, tok0:tok0 + 512] for h in range(8)], [("Kh_r", h, tc) for h in range(8)])
                      if KSUB < 5:
                          continue
                      for h in range(8):
                          G, Gk = nextG()
                          for kc in range(3):
                              sch.pe(lambda t, G=G, kc=kc, h=h: t.matmul(
                                  G[0:96, :], lhsT=wuq[:, kc, h * 96:(h + 1) * 96], rhs=cqg[:, kc, :],
                                  start=(kc == 0), stop=(kc == 2)), reads=["wuq"] + [("cqg", c) for c in range(3)],
                                  writes=[Gk])
                          sch.dve(lambda v, G=G, h=h: v.tensor_tensor(out=Qh[0:64, h, :], in0=G[0:64, :], in1=rq_bc[0:64, :],
                                                                      op=ALU.mult), reads=[Gk, "rq_bc"], writes=[("Qh_n", h)])
                          sch.dve(lambda v, G=G: v.tensor_tensor(out=zs[64:96, :], in0=G[64:96, :], in1=rq_bc[64:96, :],
                                                                 op=ALU.mult), reads=[Gk, "rq_bc"], writes=["zs"])
                          sch.pe(lambda t: t.matmul(psR[:, :], lhsT=perm16[:, :], rhs=zs[:, :], start=True,
                                                    stop=True), reads=["zs", "perm16"], writes=["psR"])
                          sch.dve(lambda v: v.tensor_tensor(out=t1[64:96, :], in0=zs[64:96, :], in1=Ct[64:96, :], op=ALU.mult),
                                  reads=["zs", ("tab", "C")], writes=["t1"])
                          sch.dve(lambda v: v.tensor_tensor(out=zs[64:96, :], in0=psR[64:96, :], in1=St[64:96, :], op=ALU.mult),
                                  reads=["psR", ("tab", "S")], writes=["zs"])
                          sch.dve(lambda v, h=h: v.tensor_tensor(out=Qh[64:96, h, :], in0=t1[64:96, :], in1=zs[64:96, :],
                                                                 op=ALU.add), reads=["t1", "zs"], writes=[("Qh_r", h)])
                          G, Gk = nextG()
                          for kc in range(2):
                              sch.pe(lambda t, G=G, kc=kc, h=h: t.matmul(
                                  G[0:64, :], lhsT=wukvk[:, kc, h * 64:(h + 1) * 64], rhs=ckvg[:, kc, :],
                                  start=(kc == 0), stop=(kc == 1)), reads=["wukvk", ("ckvg", 0), ("ckvg", 1)], writes=[Gk])
                          sch.dve(lambda v, G=G, h=h: v.tensor_tensor(
                              out=Kh[0:64, h, tok0:tok0 + 512], in0=G[0:64, :], in1=rkv_bc[0:64, :], op=ALU.mult),
                              reads=[Gk, "rkv_bc"], writes=[("Kh_n", h, tc)])
                      if KSUB < 6:
                          continue
                      for j in range(4):
                          tt = tc * 4 + j
                          G, Gk = nextG()
                          for kc in range(2):
                              sch.pe(lambda t, G=G, kc=kc, j=j: t.matmul(
                                  G[:, :], lhsT=ckvg[:, kc, j * 128:(j + 1) * 128], rhs=wukvv[:, kc, :],
                                  start=(kc == 0), stop=(kc == 1)), reads=["wukvv", ("ckvg", 0), ("ckvg", 1)], writes=[Gk])
                          sch.dve(lambda v, G=G, tt=tt, j=j: v.tensor_scalar(
                              out=Vb[:, tt, :], in0=G[:, :], scalar1=rkv_tok[:, j:j + 1], scalar2=None, op0=ALU.mult),
                              reads=[Gk, "rkv_tok"], writes=[("Vb", tt)])
                      if KSUB < 7:
                          continue
                      nst = (tc + 1) * 4
                      heads = []
                      for h in range(8):
                          hp = (h % 2) * 64
                          cc = h // 2
                          heads.append(dict(
                              kT_fn=lambda st, h=h: Kh[0:96, h, st * 128:(st + 1) * 128],
                              qT_fn=lambda h=h: Qh[0:96, h, :],
                              kq_reads=[("Kh_n", h, c) for c in range(tc + 1)] + [("Kh_r", h, c) for c in range(tc + 1)] +
                                       [("Qh_n", h), ("Qh_r", h)],
                              bias_fn=lambda st, tc=tc: ((triT[:, st - tc * 4, :], ["triT"]) if st >= tc * 4 else None),
                              v_fn=lambda st, cc=cc: Vb[:, st, cc * 128:(cc + 1) * 128],
                              v_reads=[("Vb", t_) for t_ in range(nst)],
                              rows=(hp, hp + 64),
                              out_ap=obT[hp:hp + 64, cc, tok0:tok0 + 512], out_key=("obT", cc, tc, h % 2)))
                      attn_chunk(nst, heads, psG, psO, psD, pT, rden, float(96 ** -0.5))
                  if "obT" in dbg_out:
                      for cc in range(4):
                          sch.dve(lambda v, cc=cc: v.tensor_copy(out=sqv[:, :, :].rearrange("p a b -> p (a b)"),
                                                                 in_=obT[:, cc, 0:1024]),
                                  reads=[("obT", cc, c, q) for c in range(4) for q in range(2)], writes=["sqv"])
                          dump("obT", sqv[:, :, :].rearrange("p a b -> p (a b)"), ["sqv"], rows=(cc * 128, (cc + 1) * 128))
                  sch.flush()
                  if stop_after == 2:
                      raise _Stop()

              p3 = ExitStack()
              with p3:
                  gT = sb("gT", [128, 16, 512], BF16, p3)
                  mixT = sb("mixT", [128, 8, 512], BF16, p3)
                  m0 = sb("m0", [128, 512], F32, p3)
                  mm0 = [m0, sb("m0b", [128, 512], F32, p3)]
                  m1 = sb("m1", [128, 512], F32, p3)
                  bgate = sb("bgate", [128, 16], F32, p3)
                  xt = [sb("xt%d" % i, [128, D], F32, p3) for i in range(2)]
                  hpre2 = [sb("hpre%d" % i, [128, D], F32, p3) for i in range(2)]
                  ustage = [sb("ustage%d" % i, [128, D], F32, p3) for i in range(4)]
                  hh = [sb("hh%d" % i, [128, D], F32, p3) for i in range(2)]
                  junk2 = [sb("junk3_%d" % i, [128, D], F32, p3) for i in range(2)]
                  g1bc = sb("g1bc", [128, D], F32, p3)
                  b1bc = sb("b1bc", [128, D], F32, p3)
                  st2 = [sb("st3_%d" % i, [128, 8], F32, p3) for i in range(2)]
                  hTf2 = [sb("hTf%d" % i, [128, 8, 128], F32, p3) for i in range(2)]
                  wrt = sb("wrt", [128, 8, E], F32, p3)
                  brbc = sb("brbc", [128, E], F32, p3)
                  lg2 = [sb("lg%d" % i, [128, E], F32, p3) for i in range(2)]
                  m82 = [sb("m8_%d" % i, [128, 8], F32, p3) for i in range(2)]
                  ex2 = [sb("ex%d" % i, [128, E], F32, p3) for i in range(2)]
                  msk2 = [sb("msk%d" % i, [128, E], F32, p3) for i in range(2)]
                  wring = [sb("wring3_%d" % i, [128, 8, 256], BF16, p3) for i in range(3)]
                  psG = [psum("psG3_%d" % i, [128, 512], F32, p3) for i in range(2)]
                  psU = [psum("psU3_%d" % i, [128, 512], F32, p3) for i in range(2)]
                  psTf2 = [psum("psTf%d" % i, [128, 512], F32, p3) for i in range(2)]
                  psL32 = [psum("psL3_%d" % i, [128, 512], F32, p3) for i in range(2)]

                  sch.dma("sp", bgate[:], bgate_h.ap(), key="c1", writes=["bgate"])
                  sch.dma("sp", g1bc[:], bass.AP(ln1g_h, 0, [[0, 128], [1, D]]), key="c1", writes=["g1bc"])
                  sch.dma("sp", b1bc[:], bass.AP(ln1b_h, 0, [[0, 128], [1, D]]), key="c1", writes=["b1bc"])
                  sch.dma("sp", wrt[:], wr_h.ap().rearrange("(kc p) n -> p kc n", p=128), key="c1", writes=["wrt"])
                  sch.dma("sp", brbc[:], bass.AP(br_h, 0, [[0, 128], [1, E]]), key="c1", writes=["brbc"])

                  woa_v = woa_h.ap().rearrange("(kc p) n -> p kc n", p=128)
                  wob_v = wob_h.ap().rearrange("(kc p) n -> p kc n", p=128)
                  wout_v = wout_h.ap().rearrange("(kc p) n -> p kc n", p=128)

                  def ld3(view, nk, c0):
                      def f(slot):
                          return [(slot[:, 0:nk, :], view[:, :, c0:c0 + 256])]
                      return f

                  loads3 = []
                  for tc in range(4):
                      for g in range(4):
                          loads3 += [ld3(w_in_v, 8, OFF_G + g * 256), ld3(w_in_v, 8, OFF_G + 1024 + g * 256),
                                     ld3(woa_v, 4, g * 256), ld3(wob_v, 4, g * 256)]
                      for g in range(4):
                          loads3 += [ld3(wout_v, 8, g * 256)]
                  ws3 = Stream(sch, "wring3", wring, loads3)
                  gcount = [0]

                  def nextG():
                      i = gcount[0] % 2
                      gcount[0] += 1
                      return psG[i], ("psG", i)

                  for tc in range(4):
                      tok0 = tc * 512
                      for g in range(4):
                          for gi in range(2):
                              slot, sk = ws3.next()
                              for c2 in range(2):
                                  fc = g * 2 + c2
                                  G, Gk = nextG()
                                  for kc in range(8):
                                      sch.pe(lambda t, G=G, kc=kc, c2=c2, slot=slot: t.matmul(
                                          G[:, :], lhsT=slot[:, kc, c2 * 128:(c2 + 1) * 128], rhs=XH[:, kc, tok0:tok0 + 512],
                                          start=(kc == 0), stop=(kc == 7)), reads=[sk, ("XH", tc)], writes=[Gk])
                                  col = gi * 8 + fc
                                  sch.act(lambda a, G=G, col=col: a.activation(
                                      out=gT[:, col, :], in_=G[:, :], func=AF.Sigmoid, bias=bgate[:, col:col + 1], scale=1.0),
                                      reads=[Gk, "bgate"], writes=[("gT", col)])
                          for (src, srck, gi) in ((oaT, "oaT", 0), (obT, "obT", 1)):
                              slot, sk = ws3.next()
                              for c2 in range(2):
                                  fc = g * 2 + c2
                                  G, Gk = nextG()
                                  for kc in range(4):
                                      sch.pe(lambda t, G=G, kc=kc, c2=c2, slot=slot, src=src: t.matmul(
                                          G[:, :], lhsT=slot[:, kc, c2 * 128:(c2 + 1) * 128], rhs=src[:, kc, tok0:tok0 + 512],
                                          start=(kc == 0), stop=(kc == 3)), reads=[sk, srck], writes=[Gk])
                                  col = gi * 8 + fc
                                  if gi == 0:
                                      sch.dve(lambda v, G=G, col=col, c2=c2: v.tensor_tensor(
                                          out=mm0[c2][:, :], in0=G[:, :], in1=gT[:, col, :], op=ALU.mult),
                                          reads=[Gk, ("gT", col)], writes=[("mm0", c2)])
                                  else:
                                      sch.dve(lambda v, G=G, col=col: v.tensor_tensor(
                                          out=m1[:, :], in0=G[:, :], in1=gT[:, col, :], op=ALU.mult),
                                          reads=[Gk, ("gT", col)], writes=["m1"])
                                      sch.dve(lambda v, fc=fc, c2=c2: v.tensor_tensor(
                                          out=mixT[:, fc, :], in0=mm0[c2][:, :], in1=m1[:, :], op=ALU.add),
                                          reads=[("mm0", c2), "m1"], writes=[("mixT", fc)])
                      if tc == 0 and "mixT0" in dbg_out:
                          for fc in range(8):
                              sch.dve(lambda v, fc=fc: v.tensor_copy(out=m0[:, :], in_=mixT[:, fc, :]),
                                      reads=[("mixT", fc), ("mm0", 0)], writes=[("mm0", 0)])
                              dump("mixT0", m0[:, :], [("mm0", 0)], rows=(fc * 128, (fc + 1) * 128))
                          for col in range(16):
                              sch.dve(lambda v, col=col: v.tensor_copy(out=m1[:, :], in_=gT[:, col, :]),
                                      reads=[("gT", col), "m1"], writes=["m1"])
                              dump("gT0", m1[:, :], ["m1"], rows=(col * 128, (col + 1) * 128))
                      for g in range(4):
                          slot, sk = ws3.next()
                          for j in range(4):
                              U = psU[(g * 4 + j) % 2]
                              Uk = ("psU", (g * 4 + j) % 2)
                              for fc in range(8):
                                  sch.pe(lambda t, U=U, fc=fc, j=j, slot=slot: t.matmul(
                                      U[:, 0:256], lhsT=mixT[:, fc, j * 128:(j + 1) * 128], rhs=slot[:, fc, :],
                                      start=(fc == 0), stop=(fc == 7)), reads=[sk] + [("mixT", f) for f in range(8)],
                                      writes=[Uk])
                              sch.act(lambda a, U=U, g=g, j=j: a.copy(out=ustage[j][:, g * 256:(g + 1) * 256], in_=U[:, 0:256]),
                                      reads=[Uk], writes=[("ustage", j, g)])
                      def tail_A(j):
                          tt = tc * 4 + j
                          q = tt % 2
                          X, Xk, H, Hk = xt[q], ("xt", q), hh[q], ("hh", q)
                          sch.dma("sp", X[:], x_h.ap()[tt * 128:(tt + 1) * 128, :], key=("xt", q), writes=[Xk])
                          sch.dve(lambda v: v.scalar_tensor_tensor(
                              out=hpre2[q][:, :], in0=X[:, :], scalar=DN_ALPHA, in1=ustage[j][:, :], op0=ALU.mult, op1=ALU.add),
                              reads=[Xk] + [("ustage", j, g) for g in range(4)], writes=[("hpre", q)])
                          layer_norm(sch, hpre2[q], ("hpre", q), H, Hk, junk2[q], ("junk", q), st2[q], ("st", q), g1bc, "g1bc",
                                     b1bc, "b1bc", epsln)
                          if tt == 0:
                              dump("h0", H[:, :], [Hk])
                          sch.dma("sp", hs_h.ap()[tt * 128:(tt + 1) * 128, :], H[:, :], key=("hst", q), reads=[Hk])
                          for half in range(2):
                              for k in range(4):
                                  kc = half * 4 + k
                                  sch.pe(lambda t, kc=kc, k=k: t.transpose(
                                      out=psTf2[q][:, k * 128:(k + 1) * 128], in_=H[:, kc * 128:(kc + 1) * 128],
                                      identity=identf[:, :]), reads=[Hk, "identf"], writes=[("psTf", q)])
                              sch.act(lambda a, half=half: a.copy(
                                  out=hTf2[q][:, half * 4:half * 4 + 4, :],
                                  in_=psTf2[q][:, :].rearrange("p (k t) -> p k t", k=4)),
                                  reads=[("psTf", q)], writes=[("hTf", q, half)])
                              sch.dve(lambda v, half=half: v.tensor_copy(
                                  out=XH[:, half * 4:half * 4 + 4, tt * 128:(tt + 1) * 128],
                                  in_=psTf2[q][:, :].rearrange("p (k t) -> p k t", k=4)),
                                  reads=[("psTf", q), ("hTf", q, half)], writes=[("XH", tc)])

                      def tail_B(j):
                          tt = tc * 4 + j
                          q = tt % 2
                          PL, PLk = psL32[q], ("psL3", q)
                          LG, M8, EX, MSK = lg2[q], m82[q], ex2[q], msk2[q]
                          rk = ("rt", q)
                          for kc in range(8):
                              sch.pe(lambda t, kc=kc: t.matmul(PL[:, 0:E], lhsT=hTf2[q][:, kc, :], rhs=wrt[:, kc, :],
                                                               start=(kc == 0), stop=(kc == 7)),
                                     reads=[("hTf", q, 0), ("hTf", q, 1), "wrt"], writes=[PLk])
                          sch.dve(lambda v: v.tensor_tensor(out=LG[:, :], in0=PL[:, 0:E], in1=brbc[:, :], op=ALU.add),
                                  reads=[PLk, "brbc"], writes=[rk])
                          sch.dve(lambda v: v.max(out=M8[:, :], in_=LG[:, :]), reads=[rk], writes=[rk])
                          sch.dve(lambda v: v.tensor_scalar(out=MSK[:, :], in0=LG[:, :], scalar1=M8[:, 3:4], scalar2=None,
                                                            op0=ALU.is_ge), reads=[rk], writes=[rk])
                          sch.dve(lambda v: v.tensor_scalar(out=EX[:, :], in0=LG[:, :], scalar1=M8[:, 0:1], scalar2=None,
                                                            op0=ALU.subtract), reads=[rk], writes=[rk])
                          sch.act(lambda a: a.activation(out=EX[:, :], in_=EX[:, :], func=AF.Exp), reads=[rk], writes=[rk])
                          sch.dve(lambda v: v.tensor_tensor(out=EX[:, :], in0=EX[:, :], in1=MSK[:, :], op=ALU.mult),
                                  reads=[rk], writes=[rk])
                          sch.dve(lambda v: v.tensor_reduce(out=M8[:, 4:5], in_=EX[:, :], axis=AXX, op=ALU.add),
                                  reads=[rk], writes=[rk])
                          sch.dve(lambda v: v.reciprocal(out=M8[:, 5:6], in_=M8[:, 4:5]), reads=[rk], writes=[rk])
                          sch.dve(lambda v: v.tensor_scalar(out=comb[:, tt, :], in0=EX[:, :], scalar1=M8[:, 5:6],
                                                            scalar2=None, op0=ALU.mult), reads=[rk], writes=[("comb", tt)])

                      tail_A(0)
                      for j in range(1, 4):
                          tail_A(j)
                          tail_B(j - 1)
                      tail_B(3)
                  if "comb" in dbg_out:
                      dump("comb", comb[:, :, :].rearrange("p a b -> p (a b)"), [("comb", t_) for t_ in range(16)])
                  sch.flush()
                  if stop_after == 3:
                      raise _Stop()
          p4 = ExitStack()
          with p4:
              acc = sb("acc", [128, 16, D], F32, p4)
              hid = sb("hid", [128, 8, S], BF16, p4)
              bup = sb("bup", [128, E * 16], F32, p4)
              bdn = sb("bdn", [E, D], F32, p4)
              combT = sb("combT", [E, 128], F32, p4)
              gt = [sb("gt%d" % i, [128, 512], F32, p4) for i in range(2)]
              sg = [sb("sg%d" % i, [128, 512], F32, p4) for i in range(2)]
              lt = [sb("lt%d" % i, [128, 512], F32, p4) for i in range(2)]
              g2bc = sb("g2bc", [128, D], F32, p4)
              b2bc = sb("b2bc", [128, D], F32, p4)
              junk = sb("junk4", [128, D], F32, p4)
              yo = [sb("yo%d" % i, [128, D], F32, p4) for i in range(2)]
              st_ = sb("st4", [128, 8], F32, p4)
              wring = [sb("wring4_%d" % i, [128, 8, 512], BF16, p4) for i in range(3)]
              psA = [psum("psA%d" % i, [128, 512], F32, p4) for i in range(4)]
              psY = [psum("psY%d" % i, [128, 512], F32, p4) for i in range(2)]
              psC = psum("psC", [128, 512], F32, p4)

              sch.dma("sp", bup[:], bup_h.ap(), key="c1", writes=["bup"])
              sch.dma("sp", bdn[:], bdn_h.ap(), key="c1", writes=["bdn"])
              sch.dma("sp", g2bc[:], bass.AP(ln2g_h, 0, [[0, 128], [1, D]]), key="c1", writes=["g2bc"])
              sch.dma("sp", b2bc[:], bass.AP(ln2b_h, 0, [[0, 128], [1, D]]), key="c1", writes=["b2bc"])
              for tt in range(16):
                  Y = yo[tt % 2]
                  Yk = ("yo", tt % 2)
                  sch.dma("sp", Y[:], hs_h.ap()[tt * 128:(tt + 1) * 128, :], key=("yo", tt % 2), writes=[Yk])
                  sch.pe(lambda t, tt=tt: t.transpose(out=psC[0:E, 0:128], in_=comb[:, tt, :], identity=identf[:, :]),
                         reads=[("comb", tt), "identf"], writes=["psC"])
                  sch.act(lambda a: a.copy(out=combT[:, :], in_=psC[0:E, 0:128]), reads=["psC"], writes=["combT"])
                  for half in range(2):
                      Yp = psY[half]
                      sch.pe(lambda t, Yp=Yp, half=half: t.matmul(Yp[:, :], lhsT=combT[:, :],
                                                                  rhs=bdn[:, half * 512:(half + 1) * 512], start=True,
                                                                  stop=True), reads=["combT", "bdn"], writes=[("psY", half)])
                      sch.dve(lambda v, Yp=Yp, half=half, tt=tt, Y=Y: v.scalar_tensor_tensor(
                          out=acc[:, tt, half * 512:(half + 1) * 512], in0=Y[:, half * 512:(half + 1) * 512], scalar=DN_ALPHA,
                          in1=Yp[:, :], op0=ALU.mult, op1=ALU.add), reads=[Yk, ("psY", half)], writes=[("acc", tt, half)])

              wup_v = [wup_h.ap()[e].rearrange("(kc p) n -> p kc n", p=128) for e in range(E)]
              wdn_v = [wdn_h.ap()[e].rearrange("(kc p) n -> p kc n", p=128) for e in range(E)]

              def ld_up(e, s):
                  def f(slot):
                      return [(slot[:, :, 0:256], wup_v[e][:, :, s * 256:(s + 1) * 256]),
                              (slot[:, :, 256:512], wup_v[e][:, :, DFF + s * 256:DFF + (s + 1) * 256])]
                  return f

              def ld_dn(e, half):
                  def f(slot):
                      return [(slot[:, :, :], wdn_v[e][:, :, half * 512:(half + 1) * 512])]
                  return f

              loads4 = []
              for e in range(E):
                  loads4 += [ld_up(e, s) for s in range(4)] + [ld_dn(e, hf) for hf in range(2)]
              ws4 = Stream(sch, "wring4", wring, loads4)
              acount = [0]

              def nextA():
                  i = acount[0] % 4
                  acount[0] += 1
                  return psA[i], ("psA", i)

              ycount = [0]
              tcount = [0]
              for e in range(E):
                  for s in range(4):
                      slot, sk = ws4.next()
                      for tb in range(4):
                          tok0 = tb * 512
                          for c2 in range(2):
                              jf = s * 2 + c2
                              Ag, Agk = nextA()
                              Al, Alk = nextA()
                              for (A, Ak, coff) in ((Ag, Agk, 0), (Al, Alk, 256)):
                                  for kc in range(8):
                                      sch.pe(lambda t, A=A, kc=kc, c2=c2, coff=coff, slot=slot: t.matmul(
                                          A[:, :], lhsT=slot[:, kc, coff + c2 * 128:coff + (c2 + 1) * 128],
                                          rhs=XH[:, kc, tok0:tok0 + 512], start=(kc == 0), stop=(kc == 7)),
                                          reads=[sk] + [("XH", tb)], writes=[Ak])
                              i = tcount[0] % 2
                              tcount[0] += 1
                              GT, SG, LT = gt[i], sg[i], lt[i]
                              bg = bup[:, e * 16 + jf:e * 16 + jf + 1]
                              bl = bup[:, e * 16 + 8 + jf:e * 16 + 8 + jf + 1]
                              sch.dve(lambda v, Ag=Ag, GT=GT, bg=bg: v.tensor_scalar(
                                  out=GT[:, :], in0=Ag[:, :], scalar1=bg, scalar2=7.0, op0=ALU.add, op1=ALU.min),
                                  reads=[Agk, "bup"], writes=[("gt", i)])
                              sch.act(lambda a, GT=GT, SG=SG: a.activation(out=SG[:, :], in_=GT[:, :], func=AF.Sigmoid,
                                                                           scale=1.702), reads=[("gt", i)], writes=[("sg", i)])
                              sch.dve(lambda v, Al=Al, LT=LT, bl=bl: v.tensor_scalar(
                                  out=LT[:, :], in0=Al[:, :], scalar1=bl, scalar2=7.0, op0=ALU.add, op1=ALU.min),
                                  reads=[Alk, "bup"], writes=[("lt", i)])
                              sch.dve(lambda g_, LT=LT: g_.tensor_scalar(
                                  out=LT[:, :], in0=LT[:, :], scalar1=-7.0, scalar2=1.0, op0=ALU.max, op1=ALU.add),
                                  reads=[("lt", i)], writes=[("lt", i)])
                              sch.dve(lambda g_, GT=GT, SG=SG: g_.tensor_tensor(out=GT[:, :], in0=GT[:, :], in1=SG[:, :],
                                                                               op=ALU.mult),
                                     reads=[("gt", i), ("sg", i)], writes=[("gt", i)])
                              sch.dve(lambda g_, GT=GT, LT=LT, jf=jf, tok0=tok0: g_.tensor_tensor(
                                  out=hid[:, jf, tok0:tok0 + 512], in0=GT[:, :], in1=LT[:, :], op=ALU.mult),
                                  reads=[("gt", i), ("lt", i)], writes=[("hid", jf, tb)])
                  for half in range(2):
                      slot, sk = ws4.next()
                      for tt in range(16):
                          yi = ycount[0] % 2
                          ycount[0] += 1
                          Yp = psY[yi]
                          for jf in range(8):
                              sch.pe(lambda t, Yp=Yp, jf=jf, tt=tt, slot=slot: t.matmul(
                                  Yp[:, :], lhsT=hid[:, jf, tt * 128:(tt + 1) * 128], rhs=slot[:, jf, :],
                                  start=(jf == 0), stop=(jf == 7)), reads=[sk] + [("hid", f, tt // 4) for f in range(8)],
                                  writes=[("psY", yi)])
                          sch.dve(lambda v, Yp=Yp, tt=tt, half=half, e=e: v.scalar_tensor_tensor(
                              out=acc[:, tt, half * 512:(half + 1) * 512], in0=Yp[:, :], scalar=comb[:, tt, e:e + 1],
                              in1=acc[:, tt, half * 512:(half + 1) * 512], op0=ALU.mult, op1=ALU.add),
                              reads=[("psY", yi), ("comb", tt), ("acc", tt, half)], writes=[("acc", tt, half)])
                          if e == E - 1 and half == 1:
                              Y = yo[tt % 2]
                              Yk = ("yo", tt % 2)
                              layer_norm(sch, acc[:, tt, :], [("acc", tt, 0), ("acc", tt, 1)], Y, Yk, junk, "junk", st_, "st",
                                         g2bc, "g2bc", b2bc, "b2bc", epsln, in_is_ap=True)
                              sch.dma("sp", y_h.ap()[tt * 128:(tt + 1) * 128, :], Y[:, :], key=("yst", tt % 2), reads=[Yk])
              sch.flush()
    except _Stop:
        pass
    return nc


def layer_norm(sch, src, src_key, dst, dst_key, junk, junk_key, st, st_key, gbc, gk, bbc, bk, eps, in_is_ap=False):
    s = src if in_is_ap else src[:, :]
    sk = src_key if isinstance(src_key, list) else [src_key]
    MEAN, SS, RSTD, NM = (st[:, i:i + 1] for i in range(4))
    sch.dve(lambda v: v.tensor_reduce(out=MEAN, in_=s, axis=AXX, op=ALU.add), reads=sk, writes=[st_key])
    sch.dve(lambda v: v.tensor_scalar(out=NM, in0=MEAN, scalar1=-1.0 / D, scalar2=None, op0=ALU.mult),
            reads=[st_key], writes=[st_key])
    sch.dve(lambda v: v.tensor_scalar(out=junk[:, :], in0=s, scalar1=NM, scalar2=None, op0=ALU.add),
            reads=sk + [st_key], writes=[junk_key])
    sch.dve(lambda v: v.memset(SS, 0.0), reads=[st_key], writes=[st_key])
    sch.act(lambda a: a.activation(out=dst[:, :], in_=junk[:, :], func=AF.Square, accum_out=SS),
            reads=[junk_key, st_key], writes=[dst_key, st_key])
    sch.act(lambda a: a.activation(out=RSTD, in_=SS, func=AF.Sqrt, bias=eps[:, 0:1], scale=1.0 / D),
            reads=[st_key, "epsln"], writes=[st_key])
    sch.dve(lambda v: v.reciprocal(out=RSTD, in_=RSTD), reads=[st_key], writes=[st_key])
    sch.dve(lambda v: v.scalar_tensor_tensor(out=dst[:, :], in0=junk[:, :], scalar=RSTD, in1=gbc[:, :], op0=ALU.mult,
                                             op1=ALU.mult), reads=[junk_key, st_key, gk], writes=[dst_key])
    sch.dve(lambda v: v.tensor_tensor(out=dst[:, :], in0=dst[:, :], in1=bbc[:, :], op=ALU.add),
            reads=[dst_key, bk], writes=[dst_key])


def _consts():
    ident = np.eye(128, dtype=np.float32)
    p64 = np.zeros((128, 128), np.float32)
    for m in range(128):
        if (m % 64) < 32:
            p64[m + 32, m] = -1.0
        else:
            p64[m - 32, m] = 1.0
    p16 = np.zeros((128, 128), np.float32)
    for m in range(64, 96):
        if (m - 64) < 16:
            p16[m + 16, m] = -1.0
        else:
            p16[m - 16, m] = 1.0
    tri = np.where(np.arange(128)[None, :] <= np.arange(128)[:, None], 0.0, NEG).astype(np.float32)
    triT = np.zeros((128, 4, 512), np.float32)
    for j in range(4):
        s = j * 128 + np.arange(128)[:, None]
        t = np.arange(512)[None, :]
        triT[:, j, :] = np.where(s <= t, 0.0, NEG)
    p = np.arange(128)
    invf = np.stack([
        (np.float32(10000.0) ** (-(p % 32).astype(np.float32) / np.float32(32))).astype(np.float32),
        (np.float32(10000.0) ** (-(p % 16).astype(np.float32) / np.float32(16))).astype(np.float32)], axis=1)
    krow = np.zeros((128, 16), np.float32)
    return dict(c_identb=ident, c_perm64=p64, c_perm16=p16, c_tri=tri, c_triT=triT.reshape(128, 2048),
                c_invf=invf.astype(np.float32), c_krow=krow)


_CACHE = {}


def kernel(x, positions, w_in, b_gate, rms_cq, rms_ckv, w_uq, w_ukv, w_o_a, w_o_b, w_out, ln1_g, ln1_b,
           w_router, b_router, w_up, b_up, w_down, b_down, ln2_g, ln2_b, _dbg=None, _stop=4):
    f = lambda a: np.ascontiguousarray(np.asarray(a), dtype=np.float32)
    x = f(x)
    positions = np.ascontiguousarray(np.asarray(positions), dtype=np.int32)
    key = (tuple(sorted((_dbg or {}).items())), _stop)
    if key not in _CACHE:
        _CACHE[key] = build_program(_dbg, _stop)
    nc = _CACHE[key]
    w_ukv_ = f(w_ukv)[0].reshape(256, 8, 128)
    shared = dict(
        w_in=f(w_in)[0],
        b_gate=np.ascontiguousarray(f(b_gate)[0].reshape(16, 128).T),
        rms_cq=np.ascontiguousarray(f(rms_cq)[0].reshape(3, 128).T),
        rms_ckv=np.ascontiguousarray(f(rms_ckv)[0].reshape(2, 128).T),
        w_uq=f(w_uq)[0],
        w_ukv_k=np.ascontiguousarray(w_ukv_[:, :, 0:64].reshape(256, 512)),
        w_ukv_v=np.ascontiguousarray(w_ukv_[:, :, 64:128].reshape(256, 512)),
        w_o_a=f(w_o_a)[0], w_o_b=f(w_o_b)[0], w_out=f(w_out)[0],
        ln1_g=f(ln1_g)[0].reshape(1, D), ln1_b=f(ln1_b)[0].reshape(1, D),
        w_router=f(w_router)[0], b_router=f(b_router)[0].reshape(1, E),
        w_up=f(w_up)[0],
        b_up=np.ascontiguousarray(f(b_up)[0].reshape(E, 16, 128).transpose(2, 0, 1).reshape(128, E * 16)),
        w_down=f(w_down)[0], b_down=f(b_down)[0],
        ln2_g=f(ln2_g)[0].reshape(1, D), ln2_b=f(ln2_b)[0].reshape(1, D),
    )
    shared.update(_consts())
    in_maps = []
    for c in range(NCORES):
        m = dict(shared)
        m["x"] = x[c]
        m["pos"] = positions[c].reshape(1, S)
        in_maps.append(m)
    res = run_bass_kernel_spmd(nc, in_maps, core_ids=list(range(NCORES)))
    out = np.stack([res.results[c]["y"] for c in range(NCORES)], axis=0).astype(np.float32)
    if _dbg:
        kernel.last_dbg = [{k: res.results[c]["dbg_" + k] for k in _dbg} for c in range(NCORES)]
    return out
```
